# Optimizing a Trainium2 kernel written in Bass

```python
import math
import jax, jax.numpy as jnp
from jax import lax
import numpy as np

D_MODEL = 1024
BATCH = 16
SEQ = 2048
DEPTH = 2

MIX_WIDTH = D_MODEL
HEAD_DIM = 64
NSA_HEADS = 8
NSA_KV_GROUPS = 2
HEADS_PER_GROUP = NSA_HEADS // NSA_KV_GROUPS
NSA_WIDTH = NSA_HEADS * HEAD_DIM
KV_WIDTH = NSA_KV_GROUPS * HEAD_DIM
CONV_WIDTH = MIX_WIDTH - NSA_WIDTH
CONV_GROUPS = CONV_WIDTH // HEAD_DIM
CONV_K = 3
CMP_STRIDE = 16
CMP_BLOCK = 2 * CMP_STRIDE
CMP_HIDDEN = 128
SLC_BLOCK = 64
SLC_TOPK = 8
WINDOW = 256
Q_BLOCK = 64
FORCE_BONUS = 1.0e4
N_BUCKETS = 32
MAX_DISTANCE = 128
MEM_HEADS = 4
MEM_HEAD_DIM = 64
MEM_WIDTH = MEM_HEADS * MEM_HEAD_DIM
MEM_LEN = 256
FFN_HIDDEN = -(-8 * D_MODEL // (3 * 256)) * 256
RMS_EPS = 1e-6
NEG_INF = -1e30
IN_SIZES = (NSA_WIDTH,) + (KV_WIDTH,) * 6 + (3 * NSA_HEADS,) + (CONV_WIDTH,) * 3
IN_WIDTH = sum(IN_SIZES)
IN_SPLITS = tuple(int(s) for s in np.cumsum(IN_SIZES)[:-1])

kernel_name = "hymba_nsa_shortconv_hybrid"


def rmsnorm(x, g):
    xf = x.astype(jnp.float32)
    y = xf * lax.rsqrt(jnp.mean(xf * xf, axis=-1, keepdims=True) + RMS_EPS)
    return (y * g.astype(jnp.float32)).astype(x.dtype)


def t5_bucket(dist):
    n = jnp.maximum(dist, 0)
    max_exact = N_BUCKETS // 2
    nf = jnp.maximum(n, 1).astype(jnp.float32)
    large = max_exact + (jnp.log(nf / max_exact) / math.log(MAX_DISTANCE / max_exact)
                         * (N_BUCKETS - max_exact)).astype(jnp.int32)
    large = jnp.minimum(large, N_BUCKETS - 1)
    return jnp.where(n < max_exact, n, large)


def masked_softmax(logits, mask, axis):
    maskf = mask.astype(jnp.float32)
    logits = jnp.where(mask, logits, NEG_INF)
    m = jnp.max(logits, axis=axis, keepdims=True)
    e = jnp.exp(logits - m) * maskf
    return e / jnp.maximum(jnp.sum(e, axis=axis, keepdims=True), 1e-30)


def compress_blocks(kv, pe, w1, w2):
    B, S, G, Dh = kv.shape
    ch = kv.reshape(B, S // CMP_STRIDE, CMP_STRIDE, G, Dh)
    blocks = jnp.concatenate([ch[:, :-1], ch[:, 1:]], axis=2)
    blocks = blocks + pe[None, None, :, None, :]
    n = blocks.shape[1]
    flat = blocks.transpose(0, 1, 3, 2, 4).reshape(B, n, G, CMP_BLOCK * Dh)
    return jax.nn.gelu(flat @ w1) @ w2


def nsa_attention(q, k_c, v_c, k_s, v_s, k_w, v_w, gates, rel_bias, pe, ck1, ck2, cv1, cv2):
    B, S, G, HPG, Dh = q.shape
    scale = Dh ** -0.5
    kcmp = compress_blocks(k_c, pe, ck1, ck2)
    vcmp = compress_blocks(v_c, pe, cv1, cv2)
    n_cmp = kcmp.shape[1]
    cmp_end = jnp.arange(n_cmp, dtype=jnp.int32) * CMP_STRIDE + CMP_BLOCK - 1
    n_slc = S // SLC_BLOCK
    topk = min(SLC_TOPK, n_slc)
    cs = np.arange(n_cmp) * CMP_STRIDE
    ss = np.arange(n_slc) * SLC_BLOCK
    ov = np.clip(np.minimum(cs[:, None] + CMP_BLOCK, ss[None, :] + SLC_BLOCK)
                 - np.maximum(cs[:, None], ss[None, :]), 0, None) / CMP_BLOCK
    ov = jnp.asarray(ov, dtype=jnp.float32)
    ks_blk = k_s.reshape(B, n_slc, SLC_BLOCK, G, Dh).transpose(0, 3, 1, 2, 4)
    vs_blk = v_s.reshape(B, n_slc, SLC_BLOCK, G, Dh).transpose(0, 3, 1, 2, 4)
    gather_blocks = jax.vmap(jax.vmap(lambda kb, ix: kb[ix]))
    rb_sel = rel_bias.reshape(G, HPG, N_BUCKETS).transpose(0, 2, 1)
    g_index = jnp.arange(G)[None, None, :, None, None]
    kw_pad = jnp.pad(k_w, ((0, 0), (WINDOW, 0), (0, 0), (0, 0)))
    vw_pad = jnp.pad(v_w, ((0, 0), (WINDOW, 0), (0, 0), (0, 0)))
    kw_len = WINDOW + Q_BLOCK
    dist_w = jnp.arange(Q_BLOCK)[:, None] + WINDOW - jnp.arange(kw_len)[None, :]
    band = (dist_w >= 0) & (dist_w < WINDOW)
    bias_w = rel_bias[:, t5_bucket(dist_w)].reshape(G, HPG, Q_BLOCK, kw_len).transpose(2, 0, 1, 3)

    def chunk(args):
        c, q_c, g_c = args
        t = c * Q_BLOCK + jnp.arange(Q_BLOCK, dtype=jnp.int32)
        dist_c = t[:, None] - cmp_end[None, :]
        bias_c = rel_bias[:, t5_bucket(dist_c)].reshape(G, HPG, Q_BLOCK, n_cmp).transpose(2, 0, 1, 3)
        lg = jnp.einsum('bqghd,bngd->bqghn', q_c, kcmp).astype(jnp.float32) * scale + bias_c
        p_cmp = masked_softmax(lg, (dist_c >= 0)[:, None, None, :], -1)
        o_cmp = jnp.einsum('bqghn,bngd->bqghd', p_cmp, vcmp.astype(jnp.float32))
        imp = jnp.einsum('bqghn,nj->bqgj', p_cmp, ov)
        blk = jnp.arange(n_slc, dtype=jnp.int32)[None, :]
        cur = (t // SLC_BLOCK)[:, None]
        valid = blk <= cur
        forced = (blk == 0) | (blk == cur) | (blk == cur - 1)
        score = jnp.where(valid[:, None, :], imp + jnp.where(forced, FORCE_BONUS, 0.0)[:, None, :], NEG_INF)
        top_val, top_idx = lax.top_k(score, topk)
        sel_ok = top_val > 0.5 * NEG_INF
        idx_bg = top_idx.transpose(0, 2, 1, 3)
        k_sel = gather_blocks(ks_blk, idx_bg)
        v_sel = gather_blocks(vs_blk, idx_bg)
        pos = top_idx[..., None] * SLC_BLOCK + jnp.arange(SLC_BLOCK, dtype=jnp.int32)
        dist_s = t[None, :, None, None, None] - pos
        mask_s = sel_ok[..., None] & (dist_s >= 0)
        bias_s = jnp.moveaxis(rb_sel[g_index, t5_bucket(dist_s)], -1, 3)
        lg = jnp.einsum('bqghd,bgqkjd->bqghkj', q_c, k_sel).astype(jnp.float32) * scale + bias_s
        p_sel = masked_softmax(lg, mask_s[:, :, :, None], (-2, -1))
        o_sel = jnp.einsum('bqghkj,bgqkjd->bqghd', p_sel, v_sel.astype(jnp.float32))
        kw_c = lax.dynamic_slice_in_dim(kw_pad, c * Q_BLOCK, kw_len, axis=1)
        vw_c = lax.dynamic_slice_in_dim(vw_pad, c * Q_BLOCK, kw_len, axis=1)
        s_pos = c * Q_BLOCK - WINDOW + jnp.arange(kw_len, dtype=jnp.int32)
        mask_w = band & (s_pos >= 0)[None, :]
        lg = jnp.einsum('bqghd,bkgd->bqghk', q_c, kw_c).astype(jnp.float32) * scale + bias_w
        p_win = masked_softmax(lg, mask_w[:, None, None, :], -1)
        o_win = jnp.einsum('bqghk,bkgd->bqghd', p_win, vw_c.astype(jnp.float32))
        out = g_c[..., 0:1] * o_cmp + g_c[..., 1:2] * o_sel + g_c[..., 2:3] * o_win
        return out.astype(q.dtype)

    nc = S // Q_BLOCK
    q_chunks = jnp.moveaxis(q.reshape(B, nc, Q_BLOCK, G, HPG, Dh), 1, 0)
    g_chunks = jnp.moveaxis(gates.reshape(B, nc, Q_BLOCK, G, HPG, 3), 1, 0)
    out = lax.map(chunk, (jnp.arange(nc, dtype=jnp.int32), q_chunks, g_chunks))
    return jnp.moveaxis(out, 0, 1).reshape(B, S, G, HPG, Dh)


def hybrid_mixer(h, w_in, w_out, gn_nsa, gn_conv, conv_w, rel_bias, pe, ck1, ck2, cv1, cv2):
    B, S, _ = h.shape
    proj = h @ w_in
    q, kc, vc, ks, vs, kw, vw, gl, cb, cc, cx = jnp.split(proj, IN_SPLITS, axis=-1)
    G, HPG, Dh = NSA_KV_GROUPS, HEADS_PER_GROUP, HEAD_DIM
    kvr = lambda a: a.reshape(B, S, G, Dh)
    gates = jax.nn.sigmoid(gl.astype(jnp.float32)).reshape(B, S, G, HPG, 3)
    o_nsa = nsa_attention(q.reshape(B, S, G, HPG, Dh), kvr(kc), kvr(vc), kvr(ks), kvr(vs),
                          kvr(kw), kvr(vw), gates, rel_bias, pe, ck1, ck2, cv1, cv2)
    o_nsa = o_nsa.reshape(B, S, NSA_WIDTH)
    u = cc * cx
    u_pad = jnp.pad(u, ((0, 0), (CONV_K - 1, 0), (0, 0)))
    y = sum(conv_w[k] * u_pad[:, CONV_K - 1 - k: CONV_K - 1 - k + S] for k in range(CONV_K))
    o_conv = cb * y
    merged = jnp.concatenate([rmsnorm(o_nsa, gn_nsa), rmsnorm(o_conv, gn_conv)], axis=-1)
    return merged @ w_out


def memory_attention(h, mem_n, wq, wkv, wo):
    B, S, _ = h.shape
    M = mem_n.shape[1]
    q = (h @ wq).reshape(B, S, MEM_HEADS, MEM_HEAD_DIM)
    k, v = jnp.split(mem_n @ wkv, 2, axis=-1)
    k = k.reshape(B, M, MEM_HEADS, MEM_HEAD_DIM)
    v = v.reshape(B, M, MEM_HEADS, MEM_HEAD_DIM)
    lg = jnp.einsum('bshd,bmhd->bhsm', q, k).astype(jnp.float32) * (MEM_HEAD_DIM ** -0.5)
    p = jax.nn.softmax(lg, axis=-1)
    o = jnp.einsum('bhsm,bmhd->bshd', p, v.astype(jnp.float32)).reshape(B, S, MEM_WIDTH)
    return o.astype(h.dtype) @ wo


def swiglu(h, wg, wu, wd):
    return (jax.nn.silu(h @ wg) * (h @ wu)) @ wd


def setup_inputs(seed: int = 0) -> dict:
    key = jax.random.key(seed)
    ks = jax.random.split(key, 24)
    f32 = jnp.float32
    nrm = lambda k, shape, fan_in: jax.random.normal(k, shape, f32) * fan_in ** -0.5
    gain = lambda k, shape: 1.0 + 0.05 * jax.random.normal(k, shape, f32)
    return {
        "x": jax.random.normal(ks[0], (BATCH, SEQ, D_MODEL), f32),
        "mem": jax.random.normal(ks[1], (BATCH, MEM_LEN, D_MODEL), f32),
        "rel_bias": 0.3 * jax.random.normal(ks[2], (NSA_HEADS, N_BUCKETS), f32),
        "norms": gain(ks[3], (DEPTH, 6, D_MODEL)),
        "mem_norm": gain(ks[4], (DEPTH, D_MODEL)),
        "gn_nsa": gain(ks[5], (DEPTH, NSA_WIDTH)),
        "gn_conv": gain(ks[6], (DEPTH, CONV_WIDTH)),
        "w_in": nrm(ks[7], (DEPTH, D_MODEL, IN_WIDTH), D_MODEL),
        "w_out": nrm(ks[8], (DEPTH, MIX_WIDTH, D_MODEL), MIX_WIDTH),
        "cmp_pe": 0.1 * jax.random.normal(ks[9], (DEPTH, CMP_BLOCK, HEAD_DIM), f32),
        "cmp_k_w1": nrm(ks[10], (DEPTH, CMP_BLOCK * HEAD_DIM, CMP_HIDDEN), CMP_BLOCK * HEAD_DIM),
        "cmp_k_w2": nrm(ks[11], (DEPTH, CMP_HIDDEN, HEAD_DIM), CMP_HIDDEN),
        "cmp_v_w1": nrm(ks[12], (DEPTH, CMP_BLOCK * HEAD_DIM, CMP_HIDDEN), CMP_BLOCK * HEAD_DIM),
        "cmp_v_w2": nrm(ks[13], (DEPTH, CMP_HIDDEN, HEAD_DIM), CMP_HIDDEN),
        "conv_w": nrm(ks[14], (DEPTH, CONV_K, CONV_WIDTH), CONV_K),
        "mem_wq": nrm(ks[15], (DEPTH, D_MODEL, MEM_WIDTH), D_MODEL),
        "mem_wkv": nrm(ks[16], (DEPTH, D_MODEL, 2 * MEM_WIDTH), D_MODEL),
        "mem_wo": nrm(ks[17], (DEPTH, MEM_WIDTH, D_MODEL), MEM_WIDTH),
        "ffn_wg": nrm(ks[18], (DEPTH, D_MODEL, FFN_HIDDEN), D_MODEL),
        "ffn_wu": nrm(ks[19], (DEPTH, D_MODEL, FFN_HIDDEN), D_MODEL),
        "ffn_wd": nrm(ks[20], (DEPTH, FFN_HIDDEN, D_MODEL), FFN_HIDDEN),
    }


def reference(x, mem, rel_bias, norms, mem_norm, gn_nsa, gn_conv, w_in, w_out, cmp_pe,
              cmp_k_w1, cmp_k_w2, cmp_v_w1, cmp_v_w2, conv_w, mem_wq, mem_wkv, mem_wo,
              ffn_wg, ffn_wu, ffn_wd):
    for l in range(DEPTH):
        h = hybrid_mixer(rmsnorm(x, norms[l, 0]), w_in[l], w_out[l], gn_nsa[l], gn_conv[l], conv_w[l],
                         rel_bias, cmp_pe[l], cmp_k_w1[l], cmp_k_w2[l], cmp_v_w1[l], cmp_v_w2[l])
        x = x + rmsnorm(h, norms[l, 1])
        h = memory_attention(rmsnorm(x, norms[l, 2]), rmsnorm(mem, mem_norm[l]),
                             mem_wq[l], mem_wkv[l], mem_wo[l])
        x = x + rmsnorm(h, norms[l, 3])
        h = swiglu(rmsnorm(x, norms[l, 4]), ffn_wg[l], ffn_wu[l], ffn_wd[l])
        x = x + rmsnorm(h, norms[l, 5])
    return x
```

```python
import math
from contextlib import ExitStack

import numpy as np
import ml_dtypes

import concourse.bass as bass
import concourse.mybir as mybir
from concourse.bass_utils import run_bass_kernel_spmd

F32 = mybir.dt.float32
BF16 = mybir.dt.bfloat16
AF = mybir.ActivationFunctionType
ALU = mybir.AluOpType

D = 1024
S = 2048
NL = 2
IN_W = 2840
FFN = 2816
KF = FFN // 128
OFF_Q, OFF_KC, OFF_VC, OFF_KS, OFF_VS, OFF_KW, OFF_VW, OFF_GL, OFF_CB, OFF_CC, OFF_CX = (
    0, 512, 640, 768, 896, 1024, 1152, 1280, 1304, 1816, 2328)
NEG = -30000.0
EPS = 1e-6
NR = 8


class P:
    def __init__(self, nc, es):
        self.nc = nc
        self.es = es
        self.eng = {'pe': nc.tensor, 'act': nc.scalar, 'dve': nc.vector, 'pool': nc.gpsimd, 'sp': nc.sync}
        self.epoch = 0
        self.semh = {}
        self.rings = {}
        self.ring_pos = {}
        for q in ('sp', 'pool'):
            self.rings[q] = []
            for i in range(NR):
                h = es.enter_context(nc.semaphore(f"d{q}{i}"))
                self.semh[('d', q, i)] = h
                self.rings[q].append(0)
            self.ring_pos[q] = 0
        self.known = {e: {} for e in self.eng}
        self.new_sems()
        self.lastw = {}
        self.readers = {}
        self.bank_i = 0
        self.n_ins = 0
        self.cur = ''
        self.labels = []

    def new_sems(self):
        self.cnt = {}
        for e in ('pe', 'act', 'dve', 'pool'):
            self.semh[('e', e)] = self.es.enter_context(self.nc.semaphore(f"s{e}{self.epoch}"))
            self.cnt[e] = 0
            for E in self.known:
                self.known[E].pop(('e', e), None)
        self.epoch += 1

    def _deps(self, r, w):
        d = {}
        for k in r:
            lw = self.lastw.get(k)
            if lw and lw[1] > d.get(lw[0], 0):
                d[lw[0]] = lw[1]
        for k in w:
            lw = self.lastw.get(k)
            if lw and lw[1] > d.get(lw[0], 0):
                d[lw[0]] = lw[1]
            for s, v in self.readers.get(k, {}).items():
                if v > d.get(s, 0):
                    d[s] = v
        return d

    def _wait(self, E, d):
        kn = self.known[E]
        for src, val in d.items():
            if E == 'pe' and src == ('e', 'pe'):
                continue
            if kn.get(src, 0) >= val:
                continue
            self.eng[E].wait_ge(self.semh[src], val)
            kn[src] = val

    def _record(self, src, v, r, w):
        for k in w:
            self.lastw[k] = (src, v)
            self.readers[k] = {}
        for k in r:
            self.readers.setdefault(k, {})[src] = v

    def emit(self, E, fn, r=(), w=(), inc=True):
        self._wait(E, self._deps(r, w))
        ins = fn(self.eng[E])
        self.labels.append((E, self.cur))
        if inc:
            self.cnt[E] += 1
            ins.then_inc(self.semh[('e', E)], 1)
            self._record(('e', E), self.cnt[E], r, w)
        else:
            assert E == 'pe'
            self._record(('e', E), self.cnt[E] + 1, r, w)
        self.n_ins += 1

    def dma(self, Q, out, in_, r=(), w=(), **kw):
        d = self._deps(r, w)
        i = self.ring_pos[Q]
        self.ring_pos[Q] = (i + 1) % NR
        src = ('d', Q, i)
        if self.rings[Q][i] > 0:
            d[src] = max(d.get(src, 0), self.rings[Q][i])
        self._wait(Q, d)
        ins = self.eng[Q].dma_start(out=out, in_=in_, **kw)
        self.rings[Q][i] += 16
        ins.then_inc(self.semh[src], 16)
        self._record(src, self.rings[Q][i], r, w)
        self.n_ins += 1

    def barrier(self):
        d = {}
        for e, c in self.cnt.items():
            if c > 0:
                d[('e', e)] = c
        for q in self.rings:
            for i, v in enumerate(self.rings[q]):
                if v > 0:
                    d[('d', q, i)] = v
        for E in self.eng:
            kn = self.known[E]
            for src, val in d.items():
                if kn.get(src, 0) >= val:
                    continue
                self.eng[E].wait_ge(self.semh[src], val)
                kn[src] = val
        self.lastw = {}
        self.readers = {}

    def next_bank(self):
        b = self.bank_i
        self.bank_i = (b + 1) % 3
        return b

    def mm(self, out, lhsT, rhs, start, stop, r, w, inc=None):
        self.emit('pe', lambda e: e.matmul(out, lhsT, rhs, start=start, stop=stop, skip_group_check=True), r=r, w=w,
                  inc=(stop if inc is None else inc))

    def tr(self, out, in_, ident, r, w):
        self.emit('pe', lambda e: e.transpose(out, in_, ident), r=r, w=w)

    def act(self, out, in_, func, r, w, bias=0.0, scale=1.0, accum_out=None):
        if accum_out is None:
            self.emit('act', lambda e: e.activation(out=out, in_=in_, func=func, bias=bias, scale=scale), r=r, w=w)
        else:
            self.emit('act', lambda e: e.activation(out=out, in_=in_, func=func, bias=bias, scale=scale,
                                                    accum_out=accum_out), r=r, w=w)

    def ts(self, out, in0, s1, s2, op0, op1, r, w, E='dve'):
        if op1 is None:
            self.emit(E, lambda e: e.tensor_scalar(out, in0, s1, None, op0), r=r, w=w)
        else:
            self.emit(E, lambda e: e.tensor_scalar(out, in0, s1, s2, op0, op1), r=r, w=w)

    def stt(self, out, in0, s, in1, op0, op1, r, w, E='dve'):
        self.emit(E, lambda e: e.scalar_tensor_tensor(out, in0, s, in1, op0, op1), r=r, w=w)

    def tt(self, out, in0, in1, op, r, w, E='dve'):
        self.emit(E, lambda e: e.tensor_tensor(out, in0, in1, op), r=r, w=w)

    def cp(self, out, in_, r, w, E='dve'):
        self.emit(E, lambda e: e.tensor_copy(out, in_), r=r, w=w)

    def memset(self, ap, val, w, E='dve'):
        self.emit(E, lambda e: e.memset(ap, val), w=w)


def _t5_bucket(dist):
    n = np.maximum(dist, 0)
    nf = np.maximum(n, 1).astype(np.float32)
    large = 16 + (np.log(nf / np.float32(16)) / np.float32(math.log(128 / 16)) * np.float32(16)).astype(np.int32)
    large = np.minimum(large, 31)
    return np.where(n < 16, n, large).astype(np.int64)


def _host_consts(rel_bias):
    rb = np.asarray(rel_bias, np.float32)
    c = {}
    c['identb'] = np.eye(128, dtype=np.float32).astype(ml_dtypes.bfloat16)
    c['identf'] = np.eye(128, dtype=np.float32)
    c['onesb'] = np.ones((128, 128), np.float32).astype(ml_dtypes.bfloat16)
    sl = np.arange(128)[:, None]
    tl = np.arange(128)[None, :]
    gw = np.empty((128, 3, 8, 128), np.float32)
    for kind, delta in enumerate((2, 1, 0)):
        d = 128 * delta + tl - sl
        valid = (d >= 0) & (d < 256)
        bk = _t5_bucket(d)
        for h in range(8):
            gw[:, kind, h, :] = np.where(valid, rb[h][bk], np.float32(NEG))
    c['gw'] = gw
    gn = np.empty((128, 2, 8, 128), np.float32)
    for kind, delta in enumerate((1, 0)):
        d = 128 * delta + tl - sl
        valid = d >= 0
        bk = _t5_bucket(d)
        for h in range(8):
            gn[:, kind, h, :] = np.where(valid, rb[h][bk], np.float32(NEG))
    c['gn'] = gn
    gc = np.empty((18, 8, 128), np.float32)
    t1 = np.arange(128)
    for h in range(8):
        gc[0, h, :] = rb[h, 31]
        for r in range(1, 17):
            m = r - 10
            d = t1 - 16 * m - 31
            gc[r, h, :] = np.where(d >= 0, rb[h][_t5_bucket(d)], np.float32(NEG))
        gc[17, h, :] = NEG
    c['gc'] = gc
    sel = np.zeros((18, 16, 128), np.float32)
    for j in range(16):
        for n in range(128):
            m = n - 8 * j
            if n == 127 or m > 6:
                row = 17
            elif m < -9:
                row = 0
            else:
                row = m + 10
            sel[row, j, n] = 1.0
    c['sel'] = sel.astype(ml_dtypes.bfloat16)
    c['rb31'] = np.ascontiguousarray(np.broadcast_to(rb[:, 31][None, :], (128, 8))).astype(np.float32)
    fb = np.zeros((128, 16, 32), np.float32)
    for j in range(16):
        t = 128 * j + np.arange(128)
        cur = (t // 64)[:, None]
        blk = np.arange(32)[None, :]
        valid = blk <= cur
        forced = (blk == 0) | (blk == cur) | (blk == cur - 1)
        fb[:, j, :] = np.where(valid, np.where(forced, 1.0e4, 0.0), -1.0e30)
    c['fb'] = fb
    e = np.zeros((32, 2048), np.float32)
    e[np.arange(2048) // 64, np.arange(2048)] = 1.0
    c['erows'] = e.astype(ml_dtypes.bfloat16)
    cs = np.arange(127) * 16
    ss_ = np.arange(32) * 64
    ov = np.clip(np.minimum(cs[:, None] + 32, ss_[None, :] + 64) - np.maximum(cs[:, None], ss_[None, :]), 0, None) / 32.0
    vo = np.zeros((128, 2, 97), np.float32)
    vo[:, :, 64] = 1.0
    vo[:127, :, 65:97] = ov[:, None, :]
    c['vcov0'] = vo.astype(ml_dtypes.bfloat16)
    return c


def _fm(v, nchunk):
    v = np.asarray(v, np.float32)
    lead = v.shape[:-1]
    v = v.reshape(lead + (nchunk, 128))
    v = np.moveaxis(v, -1, 0)
    return np.ascontiguousarray(v.reshape(128, -1))


def build_program(n_seq=2, layers=(0, 1), stop_after=None):
    nc = bass.Bass("TRN2", target_bir_lowering=False)
    dt_in = lambda name, shape, dt=F32: nc.dram_tensor(name, list(shape), dt, kind="ExternalInput").ap()
    x_d = dt_in("x", (n_seq, S, D))
    mem_d = dt_in("mem", (n_seq, 256, D))
    y_d = nc.dram_tensor("y", [n_seq, S, D], F32, kind="ExternalOutput").ap()
    w_in_d = dt_in("w_in", (NL, D, IN_W))
    w_out_d = dt_in("w_out", (NL, D, D))
    ck1_d = dt_in("cmp_k_w1", (NL, 2048, 128))
    ck2_d = dt_in("cmp_k_w2", (NL, 128, 64))
    cv1_d = dt_in("cmp_v_w1", (NL, 2048, 128))
    cv2_d = dt_in("cmp_v_w2", (NL, 128, 64))
    pe_d = dt_in("cmp_pe", (NL, 32, 64))
    mwq_d = dt_in("mem_wq", (NL, D, 256))
    mwkv_d = dt_in("mem_wkv", (NL, D, 512))
    mwo_d = dt_in("mem_wo", (NL, 256, D))
    wg_d = dt_in("ffn_wg", (NL, D, FFN))
    wu_d = dt_in("ffn_wu", (NL, D, FFN))
    wd_d = dt_in("ffn_wd", (NL, FFN, D))
    c_identb = dt_in("identb", (128, 128), BF16)
    c_identf = dt_in("identf", (128, 128))
    c_onesb = dt_in("onesb", (128, 128), BF16)
    c_gw = dt_in("gw", (128, 3, 8, 128))
    c_gn = dt_in("gn", (128, 2, 8, 128))
    c_gc = dt_in("gc", (18, 8, 128))
    c_sel = dt_in("sel", (18, 16, 128), BF16)
    c_rb31 = dt_in("rb31", (128, 8))
    c_fb = dt_in("fb", (128, 16, 32))
    c_erows = dt_in("erows", (32, 2048), BF16)
    c_vcov0 = dt_in("vcov0", (128, 2, 97), BF16)
    c_norms = dt_in("normsT", (128, NL * 6 * 8))
    c_memnorm = dt_in("memnormT", (128, NL * 8))
    c_gnnsa = dt_in("gnnsaT", (128, NL * 4))
    c_gnconv = dt_in("gnconvT", (128, NL * 4))
    c_convw = dt_in("convwT", (128, NL * 3 * 4))

    with ExitStack() as es:
        p = P(nc, es)
        _uid = [0]

        def sb(name, shape, dt=F32, st=es):
            _uid[0] += 1
            return st.enter_context(nc.sbuf_tensor(f"{name}_{_uid[0]}", list(shape), dt))
        xT = sb("xT", (128, 8, S))
        identb = sb("identb_s", (128, 128), BF16)
        identf = sb("identf_s", (128, 128))
        onesb = sb("onesb_s", (128, 128), BF16)
        wb = sb("wb_s", (128, 3, 8, 128), BF16)
        nb = sb("nb_s", (128, 2, 8, 128), BF16)
        bc = sb("bc_s", (18, 8, 128), BF16)
        sel = sb("sel_s", (18, 16, 128), BF16)
        rb31 = sb("rb31_s", (128, 8))
        fb = sb("fb_s", (128, 16, 32))
        normsT = sb("normsT_s", (128, NL * 6 * 8))
        memnormT = sb("memnormT_s", (128, NL * 8))
        gnnsaT = sb("gnnsaT_s", (128, NL * 4))
        gnconvT = sb("gnconvT_s", (128, NL * 4))
        convwT = sb("convwT_s", (128, NL * 3 * 4))
        TM = sb("TM", (128, 4, 96), BF16)
        ps = [es.enter_context(nc.psum_tensor(f"ps{i}", [128, 512], F32)) for i in range(7)]
        psb = es.enter_context(nc.psum_tensor("psb", [128, 1024], BF16))
        wfm = [None, None, None]
        wtm = [None, None]
        st = {'wfm': 0, 'wtm': 0, 'uid': 0}

        def alloc_w(stk, kc, with_tm=True, nbuf=3):
            st['uid'] += 1
            del wfm[:]
            for i in range(nbuf):
                wfm.append(sb(f"wfm{st['uid']}_{i}", (128, kc, 128), BF16, stk))
            st['wfm'] = 0
            if with_tm:
                for i in range(2):
                    wtm[i] = sb(f"wtm{st['uid']}_{i}", (128, 8, 256), BF16, stk)

        def g_norm(l, i, c):
            k = (l * 6 + i) * 8 + c
            return normsT[:, k:k + 1]

        def cpx(out, in_, r, w, E='dve'):
            if E == 'act':
                p.act(out, in_, AF.Copy, r=r, w=w)
            else:
                p.cp(out, in_, r, w, E)

        for dst, src, key in ((identb, c_identb, 'identb'), (identf, c_identf, 'identf'), (onesb, c_onesb, 'onesb'),
                              (sel, c_sel, 'sel'), (rb31, c_rb31, 'rb31'), (fb, c_fb, 'fb'),
                              (normsT, c_norms, 'normsT'), (memnormT, c_memnorm, 'memnormT'),
                              (gnnsaT, c_gnnsa, 'gnnsaT'), (gnconvT, c_gnconv, 'gnconvT'), (convwT, c_convw, 'convwT')):
            p.dma('sp', dst[:], src, w=[key])
        p.dma('pool', wb[:], c_gw, w=['wb'])
        p.dma('pool', bc[:], c_gc, w=['bc'])
        with ExitStack() as es0:
            gnst = sb("gnst", (128, 2, 8, 128), F32, es0)
            p.dma('sp', gnst[:], c_gn, w=['gnst'])
            for h in range(8):
                p.ts(nb[:, :, h, :], gnst[:, :, h, :], rb31[:, h:h + 1], None, ALU.subtract, None,
                     r=['gnst', 'rb31'], w=['nb'])
            p.memset(TM[:], 0.0, w=[('TM', i) for i in range(4)])
            p.barrier()

        def load_fm(src_ap, KC, M, Q='pool'):
            b = st['wfm']
            st['wfm'] = (b + 1) % len(wfm)
            p.dma(Q, wfm[b][:, 0:KC, 0:M], src_ap, w=[('wfm', b)])
            return b

        def prefetch_fm(specs, KC, n=2):
            return [load_fm(sp[0], KC, sp[1]) for sp in specs[:n]]

        def lin_fm(specs, rhs, KC, ntg, evac, tgw=512, pre=None):
            p.cur = 'lin_fm'
            n = len(specs)
            bufs = list(pre) if pre else []
            dist = len(wfm) - 1
            while len(bufs) < min(dist, n):
                bufs.append(load_fm(specs[len(bufs)][0], KC, specs[len(bufs)][1]))
            for i in range(n):
                if i + dist < n:
                    bufs.append(load_fm(specs[i + dist][0], KC, specs[i + dist][1]))
                b = bufs[i]
                M = specs[i][1]
                for tg in range(ntg):
                    bank = p.next_bank()
                    for kc in range(KC):
                        ap, key = rhs(kc, tg)
                        p.mm(ps[bank][0:M, 0:tgw], wfm[b][:, kc, 0:M], ap, kc == 0, kc == KC - 1,
                             r=[('wfm', b), key], w=[('ps', bank)])
                    evac(i, tg, bank, M)

        def wview(d_ap, l, c0, M):
            return d_ap[l, :, c0:c0 + M].rearrange("(c p) m -> p c m", p=128)

        def rstd_from(bank, lnv, rstd, n):
            p.act(lnv[:, :], ps[bank][:, :], AF.Ln, r=[('ps', bank)], w=['lnv'], bias=EPS, scale=1.0 / n)
            p.act(rstd[:, :], lnv[:, :], AF.Exp, r=['lnv'], w=['rstd'], scale=-0.5)

        def prenorm(l, ni, tg, xn, sq, lnv, rstd, xn_off=0, tag='', part=None):
            p.cur = 'prenorm'
            t0 = tg * 512
            if part in (None, 'a'):
                for c in range(8):
                    p.act(sq[:, c, :], xT[:, c, t0:t0 + 512], AF.Square, r=[('xT', c, tg)], w=[('sq', c)])
            if part in (None, 'b'):
                bank = p.next_bank()
                for c in range(8):
                    p.mm(ps[bank][:, :], onesb[:, :], sq[:, c, :], c == 0, c == 7, r=['onesb', ('sq', c)],
                         w=[('ps', bank)])
                rstd_from(bank, lnv, rstd, D)
                for c in range(8):
                    p.stt(xn[:, c, xn_off:xn_off + 512], xT[:, c, t0:t0 + 512], g_norm(l, ni, c), rstd[:, :],
                          ALU.mult, ALU.mult, r=[('xT', c, tg), 'rstd', 'normsT'], w=[('xn' + tag, c)])

        def postnorm_residual(l, ni, tg, htmp, sq, lnv, rstd):
            p.cur = 'postnorm'
            t0 = tg * 512
            bank = p.next_bank()
            for c in range(8):
                p.mm(ps[bank][:, :], onesb[:, :], sq[:, c, :], c == 0, c == 7, r=['onesb', ('sq', c)], w=[('ps', bank)])
            rstd_from(bank, lnv, rstd, D)
            for c in range(8):
                p.stt(htmp[:, c, :], htmp[:, c, :], g_norm(l, ni, c), rstd[:, :], ALU.mult, ALU.mult,
                      r=[('htmp', c), 'rstd', 'normsT'], w=[('htmp', c)])
                p.tt(xT[:, c, t0:t0 + 512], xT[:, c, t0:t0 + 512], htmp[:, c, :], ALU.add,
                     r=[('xT', c, tg), ('htmp', c)], w=[('xT', c, tg)])

        def out_specs(l, wd_ap):
            return [(wd_ap[l, :, o * 128:(o + 1) * 128].rearrange("(c p) m -> p c m", p=128), 128) for o in range(8)]

        def out_proj(l, ni, tg, wd_ap, KC, rhs, htmp, sq, lnv, rstd, pre=None):
            specs = [(wd_ap[l, :, o * 128:(o + 1) * 128].rearrange("(c p) m -> p c m", p=128), 128) for o in range(8)]

            def evac(i, tg_, bank, M):
                p.act(htmp[:, i, :], ps[bank][:, :], AF.Copy, r=[('ps', bank)], w=[('htmp', i)])
                p.act(sq[:, i, :], ps[bank][:, :], AF.Square, r=[('ps', bank)], w=[('sq', i)])
            lin_fm(specs, rhs, KC, 1, evac, pre=pre)
            postnorm_residual(l, ni, tg, htmp, sq, lnv, rstd)

        def load_sequence(s):
            with ExitStack() as es1:
                xst = [sb(f"xst_l{s}_{i}", (128, 1024), F32, es1) for i in range(2)]
                for tt_ in range(16):
                    b = tt_ % 2
                    p.dma('sp', xst[b][:, :], x_d[s, tt_ * 128:(tt_ + 1) * 128, :], w=[('xst', b)])
                    for half in range(2):
                        bank = p.next_bank()
                        for cc in range(4):
                            c = half * 4 + cc
                            p.tr(ps[bank][:, cc * 128:(cc + 1) * 128], xst[b][:, c * 128:(c + 1) * 128], identf[:, :],
                                 r=[('xst', b), 'identf'], w=[('ps', bank)])
                        cpx(xT[:, half * 4:half * 4 + 4, tt_ * 128:(tt_ + 1) * 128],
                            ps[bank][:, :].rearrange("p (c t) -> p c t", c=4),
                            r=[('ps', bank)], w=[('xT', half * 4 + cc, tt_ // 4) for cc in range(4)],
                            E=('dve' if half == 0 else 'act'))
                p.barrier()

        def store_sequence(s):
            with ExitStack() as es1:
                xst = [sb(f"xst_s{s}_{i}", (128, 1024), F32, es1) for i in range(2)]
                for tt_ in range(16):
                    b = tt_ % 2
                    for half in range(2):
                        bank = p.next_bank()
                        for cc in range(4):
                            c = half * 4 + cc
                            p.tr(ps[bank][:, cc * 128:(cc + 1) * 128], xT[:, c, tt_ * 128:(tt_ + 1) * 128], identf[:, :],
                                 r=[('xT', c, tt_ // 4), 'identf'], w=[('ps', bank)])
                        cpx(xst[b][:, half * 512:(half + 1) * 512], ps[bank][:, :], r=[('ps', bank)], w=[('xst', b)],
                            E=('dve' if half == 0 else 'act'))
                    p.dma('sp', y_d[s, tt_ * 128:(tt_ + 1) * 128, :], xst[b][:, :], r=[('xst', b)], w=[('y', tt_)])
                p.barrier()

        def mixer(l):
            with ExitStack() as esm:
                alloc_w(esm, 8, nbuf=5)
                KE = sb("KE", (96, 2, S), BF16, esm)
                KwT = sb("KwT", (64, 2, S), BF16, esm)
                Vaug = sb("Vaug", (128, 16, 2, 2, 65), BF16, esm)
                KcmpT = sb("KcmpT", (64, 2, 128), BF16, esm)
                VcOv = sb("VcOv", (128, 2, 97), BF16, esm)
                xn = sb("xn", (128, 8, 512), BF16, esm)
                sq = sb("sq", (128, 8, 512), BF16, esm)
                lnv = sb("lnv", (128, 512), F32, esm)
                rstd = sb("rstd", (128, 512), F32, esm)
                for g in range(2):
                    p.dma('sp', KE[64:96, g, :], c_erows, w=[('KE', g, tg) for tg in range(4)])
                p.dma('sp', VcOv[:], c_vcov0, w=['VcOv'])
                p.memset(Vaug[:], 1.0, w=[('Vaug', t) for t in range(16)])
                p.memset(KcmpT[:], 0.0, w=['KcmpT'])

                def rhs_xn(kc, tg):
                    return xn[:, kc, :], ('xn', kc)

                with ExitStack() as esk:
                    kcvT = sb("kcvT", (128, 2, 128, 16), BF16, esk)
                    w1dup = sb("w1dup", (128, 2, 32, 128), BF16, esk)
                    w2kv = sb("w2kv", (128, 2, 64), BF16, esk)
                    peT = sb("peT", (64, 32), BF16, esk)
                    b1 = sb("b1", (128, 2), F32, esk)
                    hx = sb("hx", (128, 128), F32, esk)
                    hx2 = sb("hx2", (128, 128), F32, esk)
                    hidT = sb("hidT", (128, 128), BF16, esk)
                    def load_cmp_weights():
                        for kv, wd_ in enumerate((ck1_d, cv1_d)):
                            src = wd_[l].rearrange("(j d) c -> d j c", d=64)
                            for dup in range(2):
                                for jh in range(2):
                                    p.dma('pool', w1dup[dup * 64:(dup + 1) * 64, kv, jh * 16:(jh + 1) * 16, :],
                                          src[:, jh * 16:(jh + 1) * 16, :], w=[('w1dup', kv)])
                        p.dma('pool', w2kv[:, 0, :], ck2_d[l], w=['w2kv'])
                        p.dma('pool', w2kv[:, 1, :], cv2_d[l], w=['w2kv'])
                        p.dma('pool', peT[:, :], pe_d[l].rearrange("j d -> d j"), w=['peT'], allow_slow_non_contiguous=True)

                    xn2 = sb("xn2", (128, 8, 512), BF16, esk)
                    xns = [xn, xn2]
                    prenorm(l, 0, 0, xns[0], sq, lnv, rstd, tag='k0')
                    for tg in range(4):
                        if tg + 1 < 4:
                            prenorm(l, 0, tg + 1, xns[(tg + 1) % 2], sq, lnv, rstd, tag='k' + str((tg + 1) % 2), part='a')
                        xc = xns[tg % 2]
                        xk = 'xnk' + str(tg % 2)
                        rhs_k = (lambda kc, tg_, xc=xc, xk=xk: (xc[:, kc, :], (xk, kc)))
                        b = st['wtm']
                        st['wtm'] = 1 - b
                        p.dma('pool', wtm[b][:, :, 0:128], wview(w_in_d, l, OFF_VS, 128), w=[('wtm', b)])
                        p.dma('pool', wtm[b][:, :, 128:256], wview(w_in_d, l, OFF_VW, 128), w=[('wtm', b)])
                        specs = [(wview(w_in_d, l, OFF_KC, 128), 128), (wview(w_in_d, l, OFF_VC, 128), 128)]
                        for g in range(2):
                            specs.append((wview(w_in_d, l, OFF_KS + 64 * g, 64), 64))
                        for g in range(2):
                            specs.append((wview(w_in_d, l, OFF_KW + 64 * g, 64), 64))

                        def evac_kv(i, tg_, bank, M, tg=tg):
                            t0 = tg * 512
                            if i < 2:
                                cpx(kcvT[:, i, tg * 32:(tg + 1) * 32, :],
                                    ps[bank][:, :].rearrange("p (n r) -> p n r", r=16),
                                    r=[('ps', bank)], w=[('kcvT', i)])
                            elif i < 4:
                                cpx(KE[0:64, i - 2, t0:t0 + 512], ps[bank][0:64, :], r=[('ps', bank)],
                                    w=[('KE', i - 2, tg)], E='act')
                            else:
                                cpx(KwT[0:64, i - 4, t0:t0 + 512], ps[bank][0:64, :], r=[('ps', bank)],
                                    w=[('KwT', i - 4, tg)])
                        lin_fm(specs, rhs_k, 8, 1, evac_kv)
                        if tg == 0:
                            load_cmp_weights()
                        if tg + 1 < 4:
                            prenorm(l, 0, tg + 1, xns[(tg + 1) % 2], sq, lnv, rstd, tag='k' + str((tg + 1) % 2), part='b')
                        p.cur = 'lin_tm_v'
                        for tt_ in range(4):
                            tile = tg * 4 + tt_
                            bank = p.next_bank()
                            for kc in range(8):
                                p.mm(ps[bank][:, 0:256], xc[:, kc, tt_ * 128:(tt_ + 1) * 128], wtm[b][:, kc, :],
                                     kc == 0, kc == 7, r=[(xk, kc), ('wtm', b)], w=[('ps', bank)])
                            cpx(Vaug[:, tile, :, :, 0:64],
                                ps[bank][:, 0:256].rearrange("p (a g d) -> p a g d", a=2, g=2),
                                r=[('ps', bank)], w=[('Vaug', tile)], E=('dve' if tt_ % 2 == 0 else 'act'))
                    p.cur = 'compress'
                    for kv in range(2):
                        bank = p.next_bank()
                        for j in range(32):
                            p.mm(ps[bank][:, 0:1], w1dup[0:64, kv, j, :], peT[:, j:j + 1], j == 0, j == 31,
                                 r=[('w1dup', kv), 'peT'], w=[('ps', bank)])
                        cpx(b1[:, kv:kv + 1], ps[bank][:, 0:1], r=[('ps', bank)], w=['b1'])
                    for kv in range(2):
                        for g in range(2):
                            bank = p.next_bank()
                            rows = slice(g * 64, (g + 1) * 64)
                            for j in range(32):
                                if j < 16:
                                    rhs_ap = kcvT[rows, kv, 0:127, j]
                                else:
                                    rhs_ap = kcvT[rows, kv, 1:128, j - 16]
                                p.mm(ps[bank][:, 0:127], w1dup[rows, kv, j, :], rhs_ap, j == 0, j == 31,
                                     r=[('w1dup', kv), ('kcvT', kv)], w=[('ps', bank)])
                            p.act(hx[:, 0:127], ps[bank][:, 0:127], AF.Identity, r=[('ps', bank), 'b1'], w=['hx'],
                                  bias=b1[:, kv:kv + 1])
                            p.tt(hx2[:, 0:127], hx[:, 0:127], hx[:, 0:127], ALU.mult, r=['hx'], w=['hx2'])
                            p.ts(hx2[:, 0:127], hx2[:, 0:127], 0.044715, 1.0, ALU.mult, ALU.add, r=['hx2'], w=['hx2'])
                            p.tt(hx2[:, 0:127], hx2[:, 0:127], hx[:, 0:127], ALU.mult, r=['hx', 'hx2'], w=['hx2'])
                            p.act(hx2[:, 0:127], hx2[:, 0:127], AF.Sigmoid, r=['hx2'], w=['hx2'], scale=1.5957691216)
                            p.tt(hidT[:, 0:127], hx2[:, 0:127], hx[:, 0:127], ALU.mult, r=['hx', 'hx2'], w=['hidT'])
                            bank2 = p.next_bank()
                            if kv == 0:
                                p.mm(ps[bank2][0:64, 0:127], w2kv[:, 0, :], hidT[:, 0:127], True, True,
                                     r=['w2kv', 'hidT'], w=[('ps', bank2)])
                                cpx(KcmpT[:, g, 0:127], ps[bank2][0:64, 0:127], r=[('ps', bank2)], w=['KcmpT'])
                            else:
                                p.mm(ps[bank2][0:127, 0:64], hidT[:, 0:127], w2kv[:, 1, :], True, True,
                                     r=['w2kv', 'hidT'], w=[('ps', bank2)])
                                cpx(VcOv[0:127, g, 0:64], ps[bank2][0:127, 0:64], r=[('ps', bank2)], w=['VcOv'])
                    p.barrier()

                with ExitStack() as esq:
                    QT = sb("QT96", (96, 8, 512), BF16, esq)
                    mg = sb("mergedT", (128, 8, 512), BF16, esq)
                    gates = sb("gates", (128, 4, 24), F32, esq)
                    htmp = sb("htmp", (128, 8, 512), F32, esq)
                    u = sb("u_conv", (128, 4, 514), F32, esq)
                    ccsb = sb("ccsb", (128, 512), F32, esq)
                    ycv = sb("ycv", (128, 512), F32, esq)
                    Pt = [sb(f"Pt{i}", (128, 512), BF16, esq) for i in range(4)]
                    oacc = sb("oacc", (128, 8, 64), F32, esq)
                    onb = sb("onb", (128, 512), BF16, esq)
                    ojunk = sb("ojunk", (128, 512), BF16, esq)
                    rz = sb("rz", (128, 3, 4), F32, esq)
                    coef = sb("coef", (128, 3, 4), F32, esq)
                    score = sb("score", (128, 32), F32, esq)
                    m8 = sb("m8", (128, 8), F32, esq)
                    thr = sb("thr", (128, 1), F32, esq)
                    ssn = sb("ssn", (128, 1), F32, esq)
                    otmp = sb("otmp", (128, 4, 64), F32, esq)
                    pti = [0]
                    p.memset(u[:], 0.0, w=[('u', c4) for c4 in range(4)])
                    pend = []
                    LOOK = 2
                    uwsb = [sb(f"uwsb{i}", (128, 260), F32, esq) for i in range(2)]
                    oaccs = [oacc, sb("oacc2", (128, 8, 64), F32, esq)]

                    def push(fn):
                        pend.append(fn)
                        while len(pend) > LOOK:
                            pend.pop(0)()

                    def delay(fn, n):
                        if n == 0:
                            fn()
                        else:
                            push(lambda: delay(fn, n - 1))

                    def flush():
                        while pend:
                            pend.pop(0)()

                    def bc3(ap2, n):
                        return ap2.unsqueeze(2).broadcast_to([ap2.shape[0], ap2.shape[1], n])

                    def attn_tile(j, jl):
                        qs = slice(jl * 128, (jl + 1) * 128)
                        oa = oaccs[j % 2]
                        oak = ('oacc', j % 2)

                        def s_tile(g, lhsT, lkeys, K, extra):
                            hs = slice(4 * g, 4 * g + 4)
                            p.cur = 'S'
                            bank = p.next_bank()
                            rk = lkeys + [('QT', g)] + ([('QTm', g, jl)] if K == 96 else [])
                            p.mm(ps[bank][:, :], lhsT, QT[0:K, hs, qs], True, len(extra) == 0, r=rk, w=[('ps', bank)])
                            for ei, (el, er, ek) in enumerate(extra):
                                p.mm(ps[bank][:, :], el, er, False, ei == len(extra) - 1, r=ek, w=[('ps', bank)])
                            pi = pti[0]
                            pti[0] = (pi + 1) % 4
                            p.act(Pt[pi][:, :], ps[bank][:, :], AF.Exp, r=[('ps', bank)], w=[('Pt', pi)])
                            return pi

                        def gview(g):
                            return gates[:, jl, g * 12:(g + 1) * 12].rearrange("p (h b) -> p h b", b=3)

                        def chain(g):
                            p.cur = 'chain'
                            ucb = 3 + g
                            uc = ps[ucb][:, 0:388].rearrange("p (h c) -> p h c", h=4)
                            gsl = gview(g)
                            p.ts(rz[:, 0, :], uc[:, :, 64], 1.0e-30, None, ALU.max, None, r=[('ps', ucb)], w=['rz0'])
                            p.emit('dve', lambda e: e.reciprocal(rz[:, 0, :], rz[:, 0, :]), r=['rz0'], w=['rz0'])
                            p.stt(score[:, :], uc[:, 0, 65:97], rz[:, 0, 0:1], fb[:, j, :], ALU.mult, ALU.add,
                                  r=[('ps', ucb), 'rz0', 'fb'], w=['score'])
                            for h in range(1, 4):
                                p.stt(score[:, :], uc[:, h, 65:97], rz[:, 0, h:h + 1], score[:, :], ALU.mult, ALU.add,
                                      r=[('ps', ucb), 'rz0', 'score'], w=['score'])
                            p.emit('dve', lambda e: e.max(m8[:, :], score[:, :]), r=['score'], w=['m8'])
                            p.ts(thr[:, :], m8[:, 7:8], -5.0e29, None, ALU.max, None, r=['m8'], w=['thr'])
                            p.ts(TM[:, (j % 2) * 2 + g, 64:96], score[:, :], thr[:, 0:1], NEG, ALU.is_lt, ALU.mult,
                                 r=['score', 'thr'], w=[('TM', (j % 2) * 2 + g)])
                            p.tt(coef[:, 0, :], rz[:, 0, :], gsl[:, :, 0], ALU.mult, r=['rz0', ('gates', jl)], w=['coef0'])
                            p.tt(oa[:, 4 * g:4 * g + 4, :], uc[:, :, 0:64], bc3(coef[:, 0, :], 64), ALU.mult,
                                 r=[('ps', ucb), 'coef0'], w=[oak + (g,)])

                        def combine(g, bi, src, skey, gcol, otmp):
                            p.cur = 'combine'
                            uu = src.rearrange("p (h c) -> p h c", h=4)
                            gsl = gview(g)
                            p.emit('dve', lambda e: e.reciprocal(rz[:, 1 + bi, :], uu[:, :, 64]), r=[skey], w=[('rz', bi)])
                            p.tt(coef[:, 1 + bi, :], rz[:, 1 + bi, :], gsl[:, :, gcol], ALU.mult,
                                 r=[('rz', bi), ('gates', jl)], w=[('coef', bi)])
                            p.tt(otmp[:, :, :], uu[:, :, 0:64], bc3(coef[:, 1 + bi, :], 64), ALU.mult,
                                 r=[skey, ('coef', bi)], w=['otmp'])
                            p.tt(oa[:, 4 * g:4 * g + 4, :], oa[:, 4 * g:4 * g + 4, :], otmp[:, :, :], ALU.add,
                                 r=['otmp', oak + (g,)], w=[oak + (g,)])

                        def finalize():
                            p.cur = 'final'
                            of = oa[:, :, :].rearrange("p h d -> p (h d)")
                            p.act(ojunk[:, :], of, AF.Square, r=[oak + (0,), oak + (1,)], w=['ojunk', 'ssn'],
                                  accum_out=ssn[:, :])
                            p.act(ssn[:, :], ssn[:, :], AF.Ln, r=['ssn'], w=['ssn'], bias=EPS, scale=1.0 / 512)
                            p.act(ssn[:, :], ssn[:, :], AF.Exp, r=['ssn'], w=['ssn'], scale=-0.5)
                            p.ts(onb[:, :], of, ssn[:, 0:1], None, ALU.mult, None, r=[oak + (0,), oak + (1,), 'ssn'],
                                 w=['onb'])
                            delay(finalize_b, 2)

                        def finalize_b():
                            p.cur = 'final'
                            for c in range(4):
                                p.tr(psb[:, 512 + c * 128:512 + (c + 1) * 128], onb[:, c * 128:(c + 1) * 128], identb[:, :],
                                     r=['onb', 'identb'], w=['psb2'])
                            for c in range(4):
                                p.ts(mg[:, c, qs], psb[:, 512 + c * 128:512 + (c + 1) * 128],
                                     gnnsaT[:, l * 4 + c:l * 4 + c + 1], None, ALU.mult, None,
                                     r=['psb2', 'gnnsaT'], w=[('mg', c)])

                        def part_cmp():
                          for g in range(2):
                            hs = slice(4 * g, 4 * g + 4)
                            pi = s_tile(g, KcmpT[0:64, g, :], ['KcmpT'], 64,
                                        [(sel[0:18, j, :], bc[0:18, hs, :], ['sel', 'bc'])])

                            def pv_c(pi=pi, g=g):
                                p.cur = 'PVc'
                                ucb = 3 + g
                                for h in range(4):
                                    p.mm(ps[ucb][:, h * 97:(h + 1) * 97], Pt[pi][:, h * 128:(h + 1) * 128], VcOv[:, g, :],
                                         h == 0, True, r=[('Pt', pi), 'VcOv'], w=[('ps', ucb)], inc=(h == 3))
                                chain(g)
                            push(pv_c)
                        def part_win():
                          for g in range(2):
                            hs = slice(4 * g, 4 * g + 4)
                            kts = list(range(max(0, j - 2), j + 1))
                            for idx, kt in enumerate(kts):
                                kind = kt - (j - 2)
                                pi = s_tile(g, KwT[0:64, g, kt * 128:(kt + 1) * 128], [('KwT', g, kt // 4)], 64,
                                            [(identb[:, :], wb[:, kind, hs, :], ['identb', 'wb'])])

                                def pv_w(pi=pi, g=g, kt=kt, first=(idx == 0), last=(idx == len(kts) - 1)):
                                    p.cur = 'PVw'
                                    for h in range(4):
                                        p.mm(ps[5][:, h * 65:(h + 1) * 65], Pt[pi][:, h * 128:(h + 1) * 128],
                                             Vaug[:, kt, 1, g, :], first and h == 0, True,
                                             r=[('Pt', pi), ('Vaug', kt)], w=[('ps', 5)], inc=(h == 3))
                                    if last:
                                        p.act(uwsb[g][:, :], ps[5][:, 0:260], AF.Copy, r=[('ps', 5)], w=[('uwsb', g)])
                                push(pv_w)
                        def part_tm():
                          p.cur = 'TM'
                          for g in range(2):
                            hs = slice(4 * g, 4 * g + 4)
                            pc = slice(g * 128, (g + 1) * 128)
                            p.tr(psb[0:96, pc], TM[:, (j % 2) * 2 + g, :], identb[:, :],
                                 r=[('TM', (j % 2) * 2 + g), 'identb'], w=[('psbT', g)])
                            p.tt(QT[64:96, hs, qs], psb[64:96, pc].unsqueeze(1).broadcast_to([32, 4, 128]),
                                 bc3(rb31[64:96, hs], 128), ALU.add, r=[('psbT', g), 'rb31'], w=[('QTm', g, jl)])

                        def part_sel():
                          for g in range(2):
                            hs = slice(4 * g, 4 * g + 4)
                            for kt in range(0, j + 1):
                                extra = []
                                if kt >= j - 1:
                                    extra = [(identb[:, :], nb[:, kt - (j - 1), hs, :], ['identb', 'nb'])]
                                pi = s_tile(g, KE[0:96, g, kt * 128:(kt + 1) * 128], [('KE', g, kt // 4)], 96, extra)

                                def pv_s(pi=pi, g=g, kt=kt, first=(kt == 0), last=(kt == j)):
                                    p.cur = 'PVs'
                                    for h in range(4):
                                        p.mm(ps[6][:, h * 65:(h + 1) * 65], Pt[pi][:, h * 128:(h + 1) * 128],
                                             Vaug[:, kt, 0, g, :], first and h == 0, True,
                                             r=[('Pt', pi), ('Vaug', kt)], w=[('ps', 6)], inc=(h == 3))
                                    if last:
                                        combine(g, 0, uwsb[g][:, :], ('uwsb', g), 2, otmp)
                                        combine(g, 1, ps[6][:, 0:260], ('ps', 6), 1, otmp)
                                        if g == 1:
                                            finalize()
                                push(pv_s)

                        return part_cmp, part_win, part_tm, part_sel
                    prenorm(l, 0, 0, xn, sq, lnv, rstd)
                    for tg in range(4):
                        b = st['wtm']
                        st['wtm'] = 1 - b
                        p.dma('pool', wtm[b][:, :, 0:24], wview(w_in_d, l, OFF_GL, 24), w=[('wtm', b)])
                        specs = [(wview(w_in_d, l, OFF_Q + 64 * h, 64), 64) for h in range(8)]
                        for c4 in range(4):
                            specs += [(wview(w_in_d, l, OFF_CC + 128 * c4, 128), 128),
                                      (wview(w_in_d, l, OFF_CX + 128 * c4, 128), 128),
                                      (wview(w_in_d, l, OFF_CB + 128 * c4, 128), 128)]
                        ocv = htmp

                        def evac_qc(i, tg_, bank, M):
                            if i < 8:
                                p.act(QT[0:64, i, :], ps[bank][0:64, :], AF.Copy, r=[('ps', bank)], w=[('QT', i // 4)],
                                      scale=0.125)
                                return
                            c4 = (i - 8) // 3
                            k3 = (i - 8) % 3
                            cw = lambda k: convwT[:, (l * 3 + k) * 4 + c4:(l * 3 + k) * 4 + c4 + 1]
                            if k3 == 0:
                                p.act(ccsb[:, :], ps[bank][:, :], AF.Copy, r=[('ps', bank)], w=['ccsb'])
                            elif k3 == 1:
                                p.tt(u[:, c4, 2:514], ccsb[:, :], ps[bank][:, :], ALU.mult, r=['ccsb', ('ps', bank)],
                                     w=[('u', c4)])
                                p.ts(ycv[:, :], u[:, c4, 2:514], cw(0), None, ALU.mult, None, r=[('u', c4), 'convwT'],
                                     w=['ycv'])
                                p.stt(ycv[:, :], u[:, c4, 1:513], cw(1), ycv[:, :], ALU.mult, ALU.add,
                                      r=[('u', c4), 'convwT', 'ycv'], w=['ycv'])
                                p.stt(ycv[:, :], u[:, c4, 0:512], cw(2), ycv[:, :], ALU.mult, ALU.add,
                                      r=[('u', c4), 'convwT', 'ycv'], w=['ycv'])
                                p.cp(u[:, c4, 0:2], u[:, c4, 512:514], r=[('u', c4)], w=[('u', c4)])
                            else:
                                p.tt(ocv[:, c4, :], ycv[:, :], ps[bank][:, :], ALU.mult, r=['ycv', ('ps', bank)],
                                     w=[('htmp', c4)])
                                p.act(sq[:, c4, :], ocv[:, c4, :], AF.Square, r=[('htmp', c4)], w=[('sq', c4)])
                        lin_fm(specs, rhs_xn, 8, 1, evac_qc)
                        p.cur = 'gates'
                        for tt_ in range(4):
                            bank = p.next_bank()
                            for kc in range(8):
                                p.mm(ps[bank][:, 0:24], xn[:, kc, tt_ * 128:(tt_ + 1) * 128], wtm[b][:, kc, 0:24],
                                     kc == 0, kc == 7, r=[('xn', kc), ('wtm', b)], w=[('ps', bank)])
                            p.act(gates[:, tt_, :], ps[bank][:, 0:24], AF.Sigmoid, r=[('ps', bank)], w=[('gates', tt_)])
                        bank = p.next_bank()
                        for c4 in range(4):
                            p.mm(ps[bank][:, :], onesb[:, :], sq[:, c4, :], c4 == 0, c4 == 3, r=['onesb', ('sq', c4)],
                                 w=[('ps', bank)])
                        rstd_from(bank, lnv, rstd, 512)
                        for c4 in range(4):
                            p.stt(mg[:, 4 + c4, :], ocv[:, c4, :], gnconvT[:, l * 4 + c4:l * 4 + c4 + 1], rstd[:, :],
                                  ALU.mult, ALU.mult, r=[('htmp', c4), 'gnconvT', 'rstd'], w=[('mg', 4 + c4)])
                        if tg + 1 < 4:
                            prenorm(l, 0, tg + 1, xn, sq, lnv, rstd)
                        pre_o = prefetch_fm(out_specs(l, w_out_d), 8)
                        parts = [attn_tile(tg * 4 + jl, jl) for jl in range(4)]
                        parts[0][0]()
                        parts[0][1]()
                        parts[0][2]()
                        for k in range(4):
                            if k < 3:
                                parts[k + 1][0]()
                            parts[k][3]()
                            if k < 3:
                                parts[k + 1][1]()
                                parts[k + 1][2]()
                        flush()
                        out_proj(l, 1, tg, w_out_d, 8, lambda kc, tg_: (mg[:, kc, :], ('mg', kc)), htmp, sq, lnv, rstd, pre=pre_o)
                    p.barrier()

        def mem_attn(l, s):
            with ExitStack() as esm:
                alloc_w(esm, 8, nbuf=5)
                memhatT = sb("memhatT", (128, 8, 256), BF16, esm)
                memn = sb("memn", (128, 8, 256), BF16, esm)
                KmT = sb("KmT", (64, 4, 256), BF16, esm)
                Vm = sb("Vm", (128, 2, 4, 65), BF16, esm)
                xn = sb("xn_m", (128, 8, 512), BF16, esm)
                sq = sb("sq_m", (128, 8, 512), BF16, esm)
                lnv = sb("lnv_m", (128, 512), F32, esm)
                rstd = sb("rstd_m", (128, 512), F32, esm)
                QmT = sb("QmT", (64, 4, 512), BF16, esm)
                omT = sb("omT", (128, 2, 512), BF16, esm)
                htmp = sb("htmp_m", (128, 8, 512), F32, esm)
                Pt = [sb(f"Ptm{i}", (128, 512), BF16, esm) for i in range(3)]
                rzm = sb("rzm", (128, 4), F32, esm)
                omb = sb("omb", (128, 256), BF16, esm)
                mst = sb("mst", (128, 1024), F32, esm)
                mss = sb("mss", (128, 1), F32, esm)
                mjunk = sb("mjunk", (128, 1024), BF16, esm)
                mhb = sb("mhb", (128, 1024), BF16, esm)
                pti = [0]
                mpend = []
                for mt in range(2):
                    p.dma('sp', mst[:, :], mem_d[s, mt * 128:(mt + 1) * 128, :], w=['mst'])
                    p.act(mjunk[:, :], mst[:, :], AF.Square, r=['mst'], w=['mjunk', 'mss'], accum_out=mss[:, :])
                    p.act(mss[:, :], mss[:, :], AF.Ln, r=['mss'], w=['mss'], bias=EPS, scale=1.0 / D)
                    p.act(mss[:, :], mss[:, :], AF.Exp, r=['mss'], w=['mss'], scale=-0.5)
                    p.ts(mhb[:, :], mst[:, :], mss[:, 0:1], None, ALU.mult, None, r=['mst', 'mss'], w=['mhb'])
                    for c in range(8):
                        p.tr(psb[:, c * 128:(c + 1) * 128], mhb[:, c * 128:(c + 1) * 128], identb[:, :],
                             r=['mhb', 'identb'], w=['psb'])
                    p.cp(memhatT[:, :, mt * 128:(mt + 1) * 128], psb[:, :].rearrange("p (c t) -> p c t", c=8),
                         r=['psb'], w=['memhatT'])
                for c in range(8):
                    p.ts(memn[:, c, :], memhatT[:, c, :], memnormT[:, l * 8 + c:l * 8 + c + 1], None, ALU.mult, None,
                         r=['memhatT', 'memnormT'], w=[('memn', c)])
                p.memset(Vm[:], 1.0, w=['Vm'])
                specs = [(wview(mwkv_d, l, 64 * h, 64), 64) for h in range(4)]

                def evac_km(i, tg_, bank, M):
                    p.cp(KmT[:, i, :], ps[bank][0:64, 0:256], r=[('ps', bank)], w=['KmT'])
                lin_fm(specs, lambda kc, tg_: (memn[:, kc, :], ('memn', kc)), 8, 1, evac_km, tgw=256)
                b = st['wtm']
                st['wtm'] = 1 - b
                p.dma('pool', wtm[b][:, :, 0:256], wview(mwkv_d, l, 256, 256), w=[('wtm', b)])
                for mt in range(2):
                    bank = p.next_bank()
                    for kc in range(8):
                        p.mm(ps[bank][:, 0:256], memn[:, kc, mt * 128:(mt + 1) * 128], wtm[b][:, kc, :], kc == 0, kc == 7,
                             r=[('memn', kc), ('wtm', b)], w=[('ps', bank)])
                    p.cp(Vm[:, mt, :, 0:64], ps[bank][:, 0:256].rearrange("p (h d) -> p h d", h=4), r=[('ps', bank)],
                         w=['Vm'])
                prenorm(l, 2, 0, xn, sq, lnv, rstd)
                for tg in range(4):
                    if tg + 1 < 4:
                        prenorm(l, 2, tg + 1, xn, sq, lnv, rstd, part='a')
                    specs = [(wview(mwq_d, l, 64 * h, 64), 64) for h in range(4)]

                    def evac_qm(i, tg_, bank, M):
                        p.act(QmT[:, i, :], ps[bank][0:64, :], AF.Copy, r=[('ps', bank)], w=['QmT'], scale=0.125)
                    lin_fm(specs, lambda kc, tg_: (xn[:, kc, :], ('xn', kc)), 8, 1, evac_qm)
                    if tg + 1 < 4:
                        prenorm(l, 2, tg + 1, xn, sq, lnv, rstd, part='b')
                    pre_o = prefetch_fm(out_specs(l, mwo_d), 2)
                    for jl in range(4):
                        qs = slice(jl * 128, (jl + 1) * 128)
                        for mt in range(2):
                            bank = p.next_bank()
                            for h in range(4):
                                p.mm(ps[bank][:, h * 128:(h + 1) * 128], KmT[:, h, mt * 128:(mt + 1) * 128], QmT[:, h, qs],
                                     h == 0, True, r=['KmT', 'QmT'], w=[('ps', bank)], inc=(h == 3))
                            pi = pti[0]
                            pti[0] = (pi + 1) % 3
                            p.act(Pt[pi][:, :], ps[bank][:, :], AF.Exp, r=[('ps', bank)], w=[('Ptm', pi)])

                            def pv_m(pi=pi, mt=mt, qs=qs):
                                for h in range(4):
                                    p.mm(ps[4][:, h * 65:(h + 1) * 65], Pt[pi][:, h * 128:(h + 1) * 128], Vm[:, mt, h, :],
                                         mt == 0 and h == 0, True, r=[('Ptm', pi), 'Vm'], w=[('ps', 4)], inc=(h == 3))
                                if mt == 1:
                                    uu = ps[4][:, 0:260].rearrange("p (h c) -> p h c", h=4)
                                    p.emit('dve', lambda e: e.reciprocal(rzm[:, :], uu[:, :, 64]), r=[('ps', 4)], w=['rzm'])
                                    p.tt(omb[:, :].rearrange("p (h d) -> p h d", h=4), uu[:, :, 0:64],
                                         rzm[:, :].unsqueeze(2).broadcast_to([128, 4, 64]), ALU.mult,
                                         r=[('ps', 4), 'rzm'], w=['omb'])
                                    for c in range(2):
                                        p.tr(psb[:, c * 128:(c + 1) * 128], omb[:, c * 128:(c + 1) * 128], identb[:, :],
                                             r=['omb', 'identb'], w=['psb'])
                                    p.cp(omT[:, :, qs], psb[:, 0:256].rearrange("p (c t) -> p c t", c=2), r=['psb'],
                                         w=[('omT', 0), ('omT', 1)])
                            mpend.append(pv_m)
                            while len(mpend) > 2:
                                mpend.pop(0)()
                    while mpend:
                        mpend.pop(0)()
                    out_proj(l, 3, tg, mwo_d, 2, lambda kc, tg_: (omT[:, kc, :], ('omT', kc)), htmp, sq, lnv, rstd, pre=pre_o)
                p.barrier()

        def ffn(l):
            with ExitStack() as esf:
                alloc_w(esf, KF, with_tm=False)
                xn = sb("xn_f", (128, 8, 1024), BF16, esf)
                sq = sb("sq_f", (128, 8, 512), BF16, esf)
                lnv = sb("lnv_f", (128, 512), F32, esf)
                rstd = sb("rstd_f", (128, 512), F32, esf)
                hT = sb("hT", (128, KF, 1024), BF16, esf)
                htmp = sb("htmp_f", (128, 8, 512), F32, esf)
                sg = [[sb(f"sg{a}{b_}", (128, 512), F32, esf) for b_ in range(2)] for a in range(2)]
                for sub in range(2):
                    prenorm(l, 4, sub, xn, sq, lnv, rstd, xn_off=sub * 512, tag=str(sub))
                for half in range(2):
                    specs = []
                    for c in range(KF):
                        specs.append((wview(wg_d, l, c * 128, 128), 128))
                        specs.append((wview(wu_d, l, c * 128, 128), 128))

                    def evac_gu(i, sub, bank, M):
                        c = i // 2
                        buf = sg[sub][c % 2]
                        key = ('sg', sub, c % 2)
                        if i % 2 == 0:
                            p.act(buf[:, :], ps[bank][:, :], AF.Silu, r=[('ps', bank)], w=[key])
                        else:
                            p.tt(hT[:, c, sub * 512:(sub + 1) * 512], buf[:, :], ps[bank][:, :], ALU.mult,
                                 r=[key, ('ps', bank)], w=[('hT', c, sub)])
                    lin_fm(specs, lambda kc, sub: (xn[:, kc, sub * 512:(sub + 1) * 512], ('xn' + str(sub), kc)), 8, 2,
                           evac_gu)
                    if half == 0:
                        for sub in range(2):
                            prenorm(l, 4, 2 + sub, xn, sq, lnv, rstd, xn_off=sub * 512, tag=str(sub))
                    for sub in range(2):
                        out_proj(l, 5, half * 2 + sub, wd_d, KF,
                                 lambda kc, tg_, sub=sub: (hT[:, kc, sub * 512:(sub + 1) * 512], ('hT', kc, sub)),
                                 htmp, sq, lnv, rstd)
                p.barrier()

        for s in range(n_seq):
            load_sequence(s)
            for l in layers:
                mixer(l)
                if stop_after == 'mixer':
                    break
                mem_attn(l, s)
                if stop_after == 'mem':
                    break
                ffn(l)
                p.new_sems()
            store_sequence(s)
            p.new_sems()
        print("program instructions:", p.n_ins)
    return nc


_PARAM_KEYS = ("w_in", "w_out", "cmp_k_w1", "cmp_k_w2", "cmp_v_w1", "cmp_v_w2", "cmp_pe", "mem_wq", "mem_wkv", "mem_wo",
               "ffn_wg", "ffn_wu", "ffn_wd")


def make_in_maps(inputs, n_cores, n_seq):
    consts = _host_consts(inputs["rel_bias"])
    shared = dict(consts)
    shared["normsT"] = _fm(inputs["norms"], 8)
    shared["memnormT"] = _fm(inputs["mem_norm"], 8)
    shared["gnnsaT"] = _fm(inputs["gn_nsa"], 4)
    shared["gnconvT"] = _fm(inputs["gn_conv"], 4)
    shared["convwT"] = _fm(inputs["conv_w"], 4)
    for k in _PARAM_KEYS:
        shared[k] = np.ascontiguousarray(np.asarray(inputs[k], np.float32))
    x = np.asarray(inputs["x"], np.float32)
    mem = np.asarray(inputs["mem"], np.float32)
    maps = []
    for c in range(n_cores):
        m = dict(shared)
        m["x"] = np.ascontiguousarray(x[c * n_seq:(c + 1) * n_seq])
        m["mem"] = np.ascontiguousarray(mem[c * n_seq:(c + 1) * n_seq])
        maps.append(m)
    return maps


def kernel(**inputs):
    n_cores = 8
    n_seq = 2
    nc = build_program(n_seq=n_seq, layers=(0, 1))
    maps = make_in_maps(inputs, n_cores, n_seq)
    res = run_bass_kernel_spmd(nc, maps, core_ids=list(range(n_cores)))
    out = np.concatenate([np.asarray(r["y"], np.float32) for r in res.results], axis=0)
    return out
```

```python
import math
from contextlib import ExitStack

import numpy as np
import ml_dtypes

import concourse.bass as bass
import concourse.mybir as mybir
from concourse.bass_utils import run_bass_kernel_spmd

F32 = mybir.dt.float32
BF16 = mybir.dt.bfloat16
AF = mybir.ActivationFunctionType
ALU = mybir.AluOpType

D = 1024
S = 2048
NL = 2
IN_W = 2840
FFN = 2816
KF = FFN // 128
OFF_Q, OFF_KC, OFF_VC, OFF_KS, OFF_VS, OFF_KW, OFF_VW, OFF_GL, OFF_CB, OFF_CC, OFF_CX = (
    0, 512, 640, 768, 896, 1024, 1152, 1280, 1304, 1816, 2328)
NEG = -30000.0
EPS = 1e-6
NR = 8


class P:
    def __init__(self, nc, es):
        self.nc = nc
        self.es = es
        self.eng = {'pe': nc.tensor, 'act': nc.scalar, 'dve': nc.vector, 'pool': nc.gpsimd, 'sp': nc.sync}
        self.epoch = 0
        self.semh = {}
        self.rings = {}
        self.ring_pos = {}
        for q in ('sp', 'pool'):
            self.rings[q] = []
            for i in range(NR):
                h = es.enter_context(nc.semaphore(f"d{q}{i}"))
                self.semh[('d', q, i)] = h
                self.rings[q].append(0)
            self.ring_pos[q] = 0
        self.known = {e: {} for e in self.eng}
        self.new_sems()
        self.lastw = {}
        self.readers = {}
        self.bank_i = 0
        self.n_ins = 0
        self.cur = ''
        self.labels = []

    def new_sems(self):
        self.cnt = {}
        for e in ('pe', 'act', 'dve', 'pool'):
            self.semh[('e', e)] = self.es.enter_context(self.nc.semaphore(f"s{e}{self.epoch}"))
            self.cnt[e] = 0
            for E in self.known:
                self.known[E].pop(('e', e), None)
        self.epoch += 1

    def _deps(self, r, w):
        d = {}
        for k in r:
            lw = self.lastw.get(k)
            if lw and lw[1] > d.get(lw[0], 0):
                d[lw[0]] = lw[1]
        for k in w:
            lw = self.lastw.get(k)
            if lw and lw[1] > d.get(lw[0], 0):
                d[lw[0]] = lw[1]
            for s, v in self.readers.get(k, {}).items():
                if v > d.get(s, 0):
                    d[s] = v
        return d

    def _wait(self, E, d):
        kn = self.known[E]
        for src, val in d.items():
            if E == 'pe' and src == ('e', 'pe'):
                continue
            if kn.get(src, 0) >= val:
                continue
            self.eng[E].wait_ge(self.semh[src], val)
            kn[src] = val

    def _record(self, src, v, r, w):
        for k in w:
            self.lastw[k] = (src, v)
            self.readers[k] = {}
        for k in r:
            self.readers.setdefault(k, {})[src] = v

    def emit(self, E, fn, r=(), w=(), inc=True):
        self._wait(E, self._deps(r, w))
        ins = fn(self.eng[E])
        self.labels.append((E, self.cur))
        if inc:
            self.cnt[E] += 1
            ins.then_inc(self.semh[('e', E)], 1)
            self._record(('e', E), self.cnt[E], r, w)
        else:
            assert E == 'pe'
            self._record(('e', E), self.cnt[E] + 1, r, w)
        self.n_ins += 1

    def dma(self, Q, out, in_, r=(), w=(), **kw):
        d = self._deps(r, w)
        i = self.ring_pos[Q]
        self.ring_pos[Q] = (i + 1) % NR
        src = ('d', Q, i)
        if self.rings[Q][i] > 0:
            d[src] = max(d.get(src, 0), self.rings[Q][i])
        self._wait(Q, d)
        ins = self.eng[Q].dma_start(out=out, in_=in_, **kw)
        self.rings[Q][i] += 16
        ins.then_inc(self.semh[src], 16)
        self._record(src, self.rings[Q][i], r, w)
        self.n_ins += 1

    def barrier(self):
        d = {}
        for e, c in self.cnt.items():
            if c > 0:
                d[('e', e)] = c
        for q in self.rings:
            for i, v in enumerate(self.rings[q]):
                if v > 0:
                    d[('d', q, i)] = v
        for E in self.eng:
            kn = self.known[E]
            for src, val in d.items():
                if kn.get(src, 0) >= val:
                    continue
                self.eng[E].wait_ge(self.semh[src], val)
                kn[src] = val
        self.lastw = {}
        self.readers = {}

    def next_bank(self):
        b = self.bank_i
        self.bank_i = (b + 1) % 3
        return b

    def mm(self, out, lhsT, rhs, start, stop, r, w, inc=None):
        self.emit('pe', lambda e: e.matmul(out, lhsT, rhs, start=start, stop=stop, skip_group_check=True), r=r, w=w,
                  inc=(stop if inc is None else inc))

    def tr(self, out, in_, ident, r, w):
        self.emit('pe', lambda e: e.transpose(out, in_, ident), r=r, w=w)

    def act(self, out, in_, func, r, w, bias=0.0, scale=1.0, accum_out=None):
        if accum_out is None:
            self.emit('act', lambda e: e.activation(out=out, in_=in_, func=func, bias=bias, scale=scale), r=r, w=w)
        else:
            self.emit('act', lambda e: e.activation(out=out, in_=in_, func=func, bias=bias, scale=scale,
                                                    accum_out=accum_out), r=r, w=w)

    def ts(self, out, in0, s1, s2, op0, op1, r, w, E='dve'):
        if op1 is None:
            self.emit(E, lambda e: e.tensor_scalar(out, in0, s1, None, op0), r=r, w=w)
        else:
            self.emit(E, lambda e: e.tensor_scalar(out, in0, s1, s2, op0, op1), r=r, w=w)

    def stt(self, out, in0, s, in1, op0, op1, r, w, E='dve'):
        self.emit(E, lambda e: e.scalar_tensor_tensor(out, in0, s, in1, op0, op1), r=r, w=w)

    def tt(self, out, in0, in1, op, r, w, E='dve'):
        self.emit(E, lambda e: e.tensor_tensor(out, in0, in1, op), r=r, w=w)

    def cp(self, out, in_, r, w, E='dve'):
        self.emit(E, lambda e: e.tensor_copy(out, in_), r=r, w=w)

    def memset(self, ap, val, w, E='dve'):
        self.emit(E, lambda e: e.memset(ap, val), w=w)


def _t5_bucket(dist):
    n = np.maximum(dist, 0)
    nf = np.maximum(n, 1).astype(np.float32)
    large = 16 + (np.log(nf / np.float32(16)) / np.float32(math.log(128 / 16)) * np.float32(16)).astype(np.int32)
    large = np.minimum(large, 31)
    return np.where(n < 16, n, large).astype(np.int64)


def _host_consts(rel_bias):
    rb = np.asarray(rel_bias, np.float32)
    c = {}
    c['identb'] = np.eye(128, dtype=np.float32).astype(ml_dtypes.bfloat16)
    c['identf'] = np.eye(128, dtype=np.float32)
    c['onesb'] = np.ones((128, 128), np.float32).astype(ml_dtypes.bfloat16)
    sl = np.arange(128)[:, None]
    tl = np.arange(128)[None, :]
    gw = np.empty((128, 3, 8, 128), np.float32)
    for kind, delta in enumerate((2, 1, 0)):
        d = 128 * delta + tl - sl
        valid = (d >= 0) & (d < 256)
        bk = _t5_bucket(d)
        for h in range(8):
            gw[:, kind, h, :] = np.where(valid, rb[h][bk], np.float32(NEG))
    c['gw'] = gw
    gn = np.empty((128, 2, 8, 128), np.float32)
    for kind, delta in enumerate((1, 0)):
        d = 128 * delta + tl - sl
        valid = d >= 0
        bk = _t5_bucket(d)
        for h in range(8):
            gn[:, kind, h, :] = np.where(valid, rb[h][bk], np.float32(NEG))
    c['gn'] = gn
    gc = np.empty((18, 8, 128), np.float32)
    t1 = np.arange(128)
    for h in range(8):
        gc[0, h, :] = rb[h, 31]
        for r in range(1, 17):
            m = r - 10
            d = t1 - 16 * m - 31
            gc[r, h, :] = np.where(d >= 0, rb[h][_t5_bucket(d)], np.float32(NEG))
        gc[17, h, :] = NEG
    c['gc'] = gc
    sel = np.zeros((18, 16, 128), np.float32)
    for j in range(16):
        for n in range(128):
            m = n - 8 * j
            if n == 127 or m > 6:
                row = 17
            elif m < -9:
                row = 0
            else:
                row = m + 10
            sel[row, j, n] = 1.0
    c['sel'] = sel.astype(ml_dtypes.bfloat16)
    c['rb31'] = np.ascontiguousarray(np.broadcast_to(rb[:, 31][None, :], (128, 8))).astype(np.float32)
    fb = np.zeros((128, 16, 32), np.float32)
    for j in range(16):
        t = 128 * j + np.arange(128)
        cur = (t // 64)[:, None]
        blk = np.arange(32)[None, :]
        valid = blk <= cur
        forced = (blk == 0) | (blk == cur) | (blk == cur - 1)
        fb[:, j, :] = np.where(valid, np.where(forced, 1.0e4, 0.0), -1.0e30)
    c['fb'] = fb
    e = np.zeros((32, 2048), np.float32)
    e[np.arange(2048) // 64, np.arange(2048)] = 1.0
    c['erows'] = e.astype(ml_dtypes.bfloat16)
    cs = np.arange(127) * 16
    ss_ = np.arange(32) * 64
    ov = np.clip(np.minimum(cs[:, None] + 32, ss_[None, :] + 64) - np.maximum(cs[:, None], ss_[None, :]), 0, None) / 32.0
    vo = np.zeros((128, 2, 97), np.float32)
    vo[:, :, 64] = 1.0
    vo[:127, :, 65:97] = ov[:, None, :]
    c['vcov0'] = vo.astype(ml_dtypes.bfloat16)
    return c


def _fm(v, nchunk):
    v = np.asarray(v, np.float32)
    lead = v.shape[:-1]
    v = v.reshape(lead + (nchunk, 128))
    v = np.moveaxis(v, -1, 0)
    return np.ascontiguousarray(v.reshape(128, -1))


def build_program(n_seq=2, layers=(0, 1), stop_after=None):
    nc = bass.Bass("TRN2", target_bir_lowering=False)
    dt_in = lambda name, shape, dt=F32: nc.dram_tensor(name, list(shape), dt, kind="ExternalInput").ap()
    x_d = dt_in("x", (n_seq, S, D))
    mem_d = dt_in("mem", (n_seq, 256, D))
    y_d = nc.dram_tensor("y", [n_seq, S, D], F32, kind="ExternalOutput").ap()
    w_in_d = dt_in("w_in", (NL, D, IN_W))
    w_out_d = dt_in("w_out", (NL, D, D))
    ck1_d = dt_in("cmp_k_w1", (NL, 2048, 128))
    ck2_d = dt_in("cmp_k_w2", (NL, 128, 64))
    cv1_d = dt_in("cmp_v_w1", (NL, 2048, 128))
    cv2_d = dt_in("cmp_v_w2", (NL, 128, 64))
    pe_d = dt_in("cmp_pe", (NL, 32, 64))
    mwq_d = dt_in("mem_wq", (NL, D, 256))
    mwkv_d = dt_in("mem_wkv", (NL, D, 512))
    mwo_d = dt_in("mem_wo", (NL, 256, D))
    wg_d = dt_in("ffn_wg", (NL, D, FFN))
    wu_d = dt_in("ffn_wu", (NL, D, FFN))
    wd_d = dt_in("ffn_wd", (NL, FFN, D))
    c_identb = dt_in("identb", (128, 128), BF16)
    c_identf = dt_in("identf", (128, 128))
    c_onesb = dt_in("onesb", (128, 128), BF16)
    c_gw = dt_in("gw", (128, 3, 8, 128))
    c_gn = dt_in("gn", (128, 2, 8, 128))
    c_gc = dt_in("gc", (18, 8, 128))
    c_sel = dt_in("sel", (18, 16, 128), BF16)
    c_rb31 = dt_in("rb31", (128, 8))
    c_fb = dt_in("fb", (128, 16, 32))
    c_erows = dt_in("erows", (32, 2048), BF16)
    c_vcov0 = dt_in("vcov0", (128, 2, 97), BF16)
    c_norms = dt_in("normsT", (128, NL * 6 * 8))
    c_memnorm = dt_in("memnormT", (128, NL * 8))
    c_gnnsa = dt_in("gnnsaT", (128, NL * 4))
    c_gnconv = dt_in("gnconvT", (128, NL * 4))
    c_convw = dt_in("convwT", (128, NL * 3 * 4))

    with ExitStack() as es:
        p = P(nc, es)
        _uid = [0]

        def sb(name, shape, dt=F32, st=es):
            _uid[0] += 1
            return st.enter_context(nc.sbuf_tensor(f"{name}_{_uid[0]}", list(shape), dt))
        xT = sb("xT", (128, 8, S))
        identb = sb("identb_s", (128, 128), BF16)
        identf = sb("identf_s", (128, 128))
        onesb = sb("onesb_s", (128, 128), BF16)
        wb = sb("wb_s", (128, 3, 8, 128), BF16)
        nb = sb("nb_s", (128, 2, 8, 128), BF16)
        bc = sb("bc_s", (18, 8, 128), BF16)
        sel = sb("sel_s", (18, 16, 128), BF16)
        rb31 = sb("rb31_s", (128, 8))
        fb = sb("fb_s", (128, 16, 32))
        normsT = sb("normsT_s", (128, NL * 6 * 8))
        memnormT = sb("memnormT_s", (128, NL * 8))
        gnnsaT = sb("gnnsaT_s", (128, NL * 4))
        gnconvT = sb("gnconvT_s", (128, NL * 4))
        convwT = sb("convwT_s", (128, NL * 3 * 4))
        TM = sb("TM", (128, 4, 96), BF16)
        ps = [es.enter_context(nc.psum_tensor(f"ps{i}", [128, 512], F32)) for i in range(7)]
        psb = es.enter_context(nc.psum_tensor("psb", [128, 1024], BF16))
        wfm = [None, None, None]
        wtm = [None, None]
        st = {'wfm': 0, 'wtm': 0, 'uid': 0}

        def alloc_w(stk, kc, with_tm=True, nbuf=3):
            st['uid'] += 1
            del wfm[:]
            for i in range(nbuf):
                wfm.append(sb(f"wfm{st['uid']}_{i}", (128, kc, 128), BF16, stk))
            st['wfm'] = 0
            if with_tm:
                for i in range(2):
                    wtm[i] = sb(f"wtm{st['uid']}_{i}", (128, 8, 256), BF16, stk)

        def g_norm(l, i, c):
            k = (l * 6 + i) * 8 + c
            return normsT[:, k:k + 1]

        def cpx(out, in_, r, w, E='dve'):
            if E == 'act':
                p.act(out, in_, AF.Copy, r=r, w=w)
            else:
                p.cp(out, in_, r, w, E)

        for dst, src, key in ((identb, c_identb, 'identb'), (identf, c_identf, 'identf'), (onesb, c_onesb, 'onesb'),
                              (sel, c_sel, 'sel'), (rb31, c_rb31, 'rb31'), (fb, c_fb, 'fb'),
                              (normsT, c_norms, 'normsT'), (memnormT, c_memnorm, 'memnormT'),
                              (gnnsaT, c_gnnsa, 'gnnsaT'), (gnconvT, c_gnconv, 'gnconvT'), (convwT, c_convw, 'convwT')):
            p.dma('sp', dst[:], src, w=[key])
        p.dma('pool', wb[:], c_gw, w=['wb'])
        p.dma('pool', bc[:], c_gc, w=['bc'])
        with ExitStack() as es0:
            gnst = sb("gnst", (128, 2, 8, 128), F32, es0)
            p.dma('sp', gnst[:], c_gn, w=['gnst'])
            for h in range(8):
                p.ts(nb[:, :, h, :], gnst[:, :, h, :], rb31[:, h:h + 1], None, ALU.subtract, None,
                     r=['gnst', 'rb31'], w=['nb'])
            p.memset(TM[:], 0.0, w=[('TM', i) for i in range(4)])
            p.barrier()

        def load_fm(src_ap, KC, M, Q='pool'):
            b = st['wfm']
            st['wfm'] = (b + 1) % len(wfm)
            p.dma(Q, wfm[b][:, 0:KC, 0:M], src_ap, w=[('wfm', b)])
            return b

        def prefetch_fm(specs, KC, n=2):
            return [load_fm(sp[0], KC, sp[1]) for sp in specs[:n]]

        def lin_fm(specs, rhs, KC, ntg, evac, tgw=512, pre=None):
            p.cur = 'lin_fm'
            n = len(specs)
            bufs = list(pre) if pre else []
            dist = len(wfm) - 1
            while len(bufs) < min(dist, n):
                bufs.append(load_fm(specs[len(bufs)][0], KC, specs[len(bufs)][1]))
            for i in range(n):
                if i + dist < n:
                    bufs.append(load_fm(specs[i + dist][0], KC, specs[i + dist][1]))
                b = bufs[i]
                M = specs[i][1]
                for tg in range(ntg):
                    bank = p.next_bank()
                    for kc in range(KC):
                        ap, key = rhs(kc, tg)
                        p.mm(ps[bank][0:M, 0:tgw], wfm[b][:, kc, 0:M], ap, kc == 0, kc == KC - 1,
                             r=[('wfm', b), key], w=[('ps', bank)])
                    evac(i, tg, bank, M)

        def wview(d_ap, l, c0, M):
            return d_ap[l, :, c0:c0 + M].rearrange("(c p) m -> p c m", p=128)

        def rstd_from(bank, lnv, rstd, n):
            p.act(lnv[:, :], ps[bank][:, :], AF.Ln, r=[('ps', bank)], w=['lnv'], bias=EPS, scale=1.0 / n)
            p.act(rstd[:, :], lnv[:, :], AF.Exp, r=['lnv'], w=['rstd'], scale=-0.5)

        def prenorm(l, ni, tg, xn, sq, lnv, rstd, xn_off=0, tag='', part=None):
            p.cur = 'prenorm'
            t0 = tg * 512
            if part in (None, 'a'):
                for c in range(8):
                    p.act(sq[:, c, :], xT[:, c, t0:t0 + 512], AF.Square, r=[('xT', c, tg)], w=[('sq', c)])
            if part in (None, 'b'):
                bank = p.next_bank()
                for c in range(8):
                    p.mm(ps[bank][:, :], onesb[:, :], sq[:, c, :], c == 0, c == 7, r=['onesb', ('sq', c)],
                         w=[('ps', bank)])
                rstd_from(bank, lnv, rstd, D)
                for c in range(8):
                    p.stt(xn[:, c, xn_off:xn_off + 512], xT[:, c, t0:t0 + 512], g_norm(l, ni, c), rstd[:, :],
                          ALU.mult, ALU.mult, r=[('xT', c, tg), 'rstd', 'normsT'], w=[('xn' + tag, c)])

        def postnorm_residual(l, ni, tg, htmp, sq, lnv, rstd):
            p.cur = 'postnorm'
            t0 = tg * 512
            bank = p.next_bank()
            for c in range(8):
                p.mm(ps[bank][:, :], onesb[:, :], sq[:, c, :], c == 0, c == 7, r=['onesb', ('sq', c)], w=[('ps', bank)])
            rstd_from(bank, lnv, rstd, D)
            for c in range(8):
                p.stt(htmp[:, c, :], htmp[:, c, :], g_norm(l, ni, c), rstd[:, :], ALU.mult, ALU.mult,
                      r=[('htmp', c), 'rstd', 'normsT'], w=[('htmp', c)])
                p.tt(xT[:, c, t0:t0 + 512], xT[:, c, t0:t0 + 512], htmp[:, c, :], ALU.add,
                     r=[('xT', c, tg), ('htmp', c)], w=[('xT', c, tg)])

        def out_specs(l, wd_ap):
            return [(wd_ap[l, :, o * 128:(o + 1) * 128].rearrange("(c p) m -> p c m", p=128), 128) for o in range(8)]

        def out_proj(l, ni, tg, wd_ap, KC, rhs, htmp, sq, lnv, rstd, pre=None):
            specs = [(wd_ap[l, :, o * 128:(o + 1) * 128].rearrange("(c p) m -> p c m", p=128), 128) for o in range(8)]

            def evac(i, tg_, bank, M):
                p.act(htmp[:, i, :], ps[bank][:, :], AF.Copy, r=[('ps', bank)], w=[('htmp', i)])
                p.act(sq[:, i, :], ps[bank][:, :], AF.Square, r=[('ps', bank)], w=[('sq', i)])
            lin_fm(specs, rhs, KC, 1, evac, pre=pre)
            postnorm_residual(l, ni, tg, htmp, sq, lnv, rstd)

        def load_sequence(s):
            with ExitStack() as es1:
                xst = [sb(f"xst_l{s}_{i}", (128, 1024), F32, es1) for i in range(2)]
                for tt_ in range(16):
                    b = tt_ % 2
                    p.dma('sp', xst[b][:, :], x_d[s, tt_ * 128:(tt_ + 1) * 128, :], w=[('xst', b)])
                    for half in range(2):
                        bank = p.next_bank()
                        for cc in range(4):
                            c = half * 4 + cc
                            p.tr(ps[bank][:, cc * 128:(cc + 1) * 128], xst[b][:, c * 128:(c + 1) * 128], identf[:, :],
                                 r=[('xst', b), 'identf'], w=[('ps', bank)])
                        cpx(xT[:, half * 4:half * 4 + 4, tt_ * 128:(tt_ + 1) * 128],
                            ps[bank][:, :].rearrange("p (c t) -> p c t", c=4),
                            r=[('ps', bank)], w=[('xT', half * 4 + cc, tt_ // 4) for cc in range(4)],
                            E=('dve' if half == 0 else 'act'))
                p.barrier()

        def store_sequence(s):
            with ExitStack() as es1:
                xst = [sb(f"xst_s{s}_{i}", (128, 1024), F32, es1) for i in range(2)]
                for tt_ in range(16):
                    b = tt_ % 2
                    for half in range(2):
                        bank = p.next_bank()
                        for cc in range(4):
                            c = half * 4 + cc
                            p.tr(ps[bank][:, cc * 128:(cc + 1) * 128], xT[:, c, tt_ * 128:(tt_ + 1) * 128], identf[:, :],
                                 r=[('xT', c, tt_ // 4), 'identf'], w=[('ps', bank)])
                        cpx(xst[b][:, half * 512:(half + 1) * 512], ps[bank][:, :], r=[('ps', bank)], w=[('xst', b)],
                            E=('dve' if half == 0 else 'act'))
                    p.dma('sp', y_d[s, tt_ * 128:(tt_ + 1) * 128, :], xst[b][:, :], r=[('xst', b)], w=[('y', tt_)])
                p.barrier()

        def mixer(l):
            with ExitStack() as esm:
                alloc_w(esm, 8, nbuf=5)
                KE = sb("KE", (96, 2, S), BF16, esm)
                KwT = sb("KwT", (64, 2, S), BF16, esm)
                Vaug = sb("Vaug", (128, 16, 2, 2, 65), BF16, esm)
                KcmpT = sb("KcmpT", (64, 2, 128), BF16, esm)
                VcOv = sb("VcOv", (128, 2, 97), BF16, esm)
                xn = sb("xn", (128, 8, 512), BF16, esm)
                sq = sb("sq", (128, 8, 512), BF16, esm)
                lnv = sb("lnv", (128, 512), F32, esm)
                rstd = sb("rstd", (128, 512), F32, esm)
                for g in range(2):
                    p.dma('sp', KE[64:96, g, :], c_erows, w=[('KE', g, tg) for tg in range(4)])
                p.dma('sp', VcOv[:], c_vcov0, w=['VcOv'])
                p.memset(Vaug[:], 1.0, w=[('Vaug', t) for t in range(16)])
                p.memset(KcmpT[:], 0.0, w=['KcmpT'])

                def rhs_xn(kc, tg):
                    return xn[:, kc, :], ('xn', kc)

                with ExitStack() as esk:
                    kcvT = sb("kcvT", (128, 2, 128, 16), BF16, esk)
                    w1dup = sb("w1dup", (128, 2, 32, 128), BF16, esk)
                    w2kv = sb("w2kv", (128, 2, 64), BF16, esk)
                    peT = sb("peT", (64, 32), BF16, esk)
                    b1 = sb("b1", (128, 2), F32, esk)
                    hx = sb("hx", (128, 128), F32, esk)
                    hx2 = sb("hx2", (128, 128), F32, esk)
                    hidT = sb("hidT", (128, 128), BF16, esk)
                    def load_cmp_weights():
                        for kv, wd_ in enumerate((ck1_d, cv1_d)):
                            src = wd_[l].rearrange("(j d) c -> d j c", d=64)
                            for dup in range(2):
                                for jh in range(2):
                                    p.dma('pool', w1dup[dup * 64:(dup + 1) * 64, kv, jh * 16:(jh + 1) * 16, :],
                                          src[:, jh * 16:(jh + 1) * 16, :], w=[('w1dup', kv)])
                        p.dma('pool', w2kv[:, 0, :], ck2_d[l], w=['w2kv'])
                        p.dma('pool', w2kv[:, 1, :], cv2_d[l], w=['w2kv'])
                        p.dma('pool', peT[:, :], pe_d[l].rearrange("j d -> d j"), w=['peT'], allow_slow_non_contiguous=True)

                    xn2 = sb("xn2", (128, 8, 512), BF16, esk)
                    xns = [xn, xn2]
                    prenorm(l, 0, 0, xns[0], sq, lnv, rstd, tag='k0')
                    for tg in range(4):
                        if tg + 1 < 4:
                            prenorm(l, 0, tg + 1, xns[(tg + 1) % 2], sq, lnv, rstd, tag='k' + str((tg + 1) % 2), part='a')
                        xc = xns[tg % 2]
                        xk = 'xnk' + str(tg % 2)
                        rhs_k = (lambda kc, tg_, xc=xc, xk=xk: (xc[:, kc, :], (xk, kc)))
                        b = st['wtm']
                        st['wtm'] = 1 - b
                        p.dma('pool', wtm[b][:, :, 0:128], wview(w_in_d, l, OFF_VS, 128), w=[('wtm', b)])
                        p.dma('pool', wtm[b][:, :, 128:256], wview(w_in_d, l, OFF_VW, 128), w=[('wtm', b)])
                        specs = [(wview(w_in_d, l, OFF_KC, 128), 128), (wview(w_in_d, l, OFF_VC, 128), 128)]
                        for g in range(2):
                            specs.append((wview(w_in_d, l, OFF_KS + 64 * g, 64), 64))
                        for g in range(2):
                            specs.append((wview(w_in_d, l, OFF_KW + 64 * g, 64), 64))

                        def evac_kv(i, tg_, bank, M, tg=tg):
                            t0 = tg * 512
                            if i < 2:
                                cpx(kcvT[:, i, tg * 32:(tg + 1) * 32, :],
                                    ps[bank][:, :].rearrange("p (n r) -> p n r", r=16),
                                    r=[('ps', bank)], w=[('kcvT', i)])
                            elif i < 4:
                                cpx(KE[0:64, i - 2, t0:t0 + 512], ps[bank][0:64, :], r=[('ps', bank)],
                                    w=[('KE', i - 2, tg)], E='act')
                            else:
                                cpx(KwT[0:64, i - 4, t0:t0 + 512], ps[bank][0:64, :], r=[('ps', bank)],
                                    w=[('KwT', i - 4, tg)])
                        lin_fm(specs, rhs_k, 8, 1, evac_kv)
                        if tg == 0:
                            load_cmp_weights()
                        if tg + 1 < 4:
                            prenorm(l, 0, tg + 1, xns[(tg + 1) % 2], sq, lnv, rstd, tag='k' + str((tg + 1) % 2), part='b')
                        p.cur = 'lin_tm_v'
                        for tt_ in range(4):
                            tile = tg * 4 + tt_
                            bank = p.next_bank()
                            for kc in range(8):
                                p.mm(ps[bank][:, 0:256], xc[:, kc, tt_ * 128:(tt_ + 1) * 128], wtm[b][:, kc, :],
                                     kc == 0, kc == 7, r=[(xk, kc), ('wtm', b)], w=[('ps', bank)])
                            cpx(Vaug[:, tile, :, :, 0:64],
                                ps[bank][:, 0:256].rearrange("p (a g d) -> p a g d", a=2, g=2),
                                r=[('ps', bank)], w=[('Vaug', tile)], E=('dve' if tt_ % 2 == 0 else 'act'))
                    p.cur = 'compress'
                    for kv in range(2):
                        bank = p.next_bank()
                        for j in range(32):
                            p.mm(ps[bank][:, 0:1], w1dup[0:64, kv, j, :], peT[:, j:j + 1], j == 0, j == 31,
                                 r=[('w1dup', kv), 'peT'], w=[('ps', bank)])
                        cpx(b1[:, kv:kv + 1], ps[bank][:, 0:1], r=[('ps', bank)], w=['b1'])
                    for kv in range(2):
                        for g in range(2):
                            bank = p.next_bank()
                            rows = slice(g * 64, (g + 1) * 64)
                            for j in range(32):
                                if j < 16:
                                    rhs_ap = kcvT[rows, kv, 0:127, j]
                                else:
                                    rhs_ap = kcvT[rows, kv, 1:128, j - 16]
                                p.mm(ps[bank][:, 0:127], w1dup[rows, kv, j, :], rhs_ap, j == 0, j == 31,
                                     r=[('w1dup', kv), ('kcvT', kv)], w=[('ps', bank)])
                            p.act(hx[:, 0:127], ps[bank][:, 0:127], AF.Identity, r=[('ps', bank), 'b1'], w=['hx'],
                                  bias=b1[:, kv:kv + 1])
                            p.tt(hx2[:, 0:127], hx[:, 0:127], hx[:, 0:127], ALU.mult, r=['hx'], w=['hx2'])
                            p.ts(hx2[:, 0:127], hx2[:, 0:127], 0.044715, 1.0, ALU.mult, ALU.add, r=['hx2'], w=['hx2'])
                            p.tt(hx2[:, 0:127], hx2[:, 0:127], hx[:, 0:127], ALU.mult, r=['hx', 'hx2'], w=['hx2'])
                            p.act(hx2[:, 0:127], hx2[:, 0:127], AF.Sigmoid, r=['hx2'], w=['hx2'], scale=1.5957691216)
                            p.tt(hidT[:, 0:127], hx2[:, 0:127], hx[:, 0:127], ALU.mult, r=['hx', 'hx2'], w=['hidT'])
                            bank2 = p.next_bank()
                            if kv == 0:
                                p.mm(ps[bank2][0:64, 0:127], w2kv[:, 0, :], hidT[:, 0:127], True, True,
                                     r=['w2kv', 'hidT'], w=[('ps', bank2)])
                                cpx(KcmpT[:, g, 0:127], ps[bank2][0:64, 0:127], r=[('ps', bank2)], w=['KcmpT'])
                            else:
                                p.mm(ps[bank2][0:127, 0:64], hidT[:, 0:127], w2kv[:, 1, :], True, True,
                                     r=['w2kv', 'hidT'], w=[('ps', bank2)])
                                cpx(VcOv[0:127, g, 0:64], ps[bank2][0:127, 0:64], r=[('ps', bank2)], w=['VcOv'])
                    p.barrier()

                with ExitStack() as esq:
                    QT = sb("QT96", (96, 8, 512), BF16, esq)
                    mg = sb("mergedT", (128, 8, 512), BF16, esq)
                    gates = sb("gates", (128, 4, 24), F32, esq)
                    htmp = sb("htmp", (128, 8, 512), F32, esq)
                    u = sb("u_conv", (128, 4, 514), F32, esq)
                    ccsb = sb("ccsb", (128, 512), F32, esq)
                    ycv = sb("ycv", (128, 512), F32, esq)
                    Pt = [sb(f"Pt{i}", (128, 512), BF16, esq) for i in range(4)]
                    oacc = sb("oacc", (128, 8, 64), F32, esq)
                    onb = sb("onb", (128, 512), BF16, esq)
                    ojunk = sb("ojunk", (128, 512), BF16, esq)
                    rz = sb("rz", (128, 3, 4), F32, esq)
                    coef = sb("coef", (128, 3, 4), F32, esq)
                    score = sb("score", (128, 32), F32, esq)
                    m8 = sb("m8", (128, 8), F32, esq)
                    thr = sb("thr", (128, 1), F32, esq)
                    ssn = sb("ssn", (128, 1), F32, esq)
                    otmp = sb("otmp", (128, 4, 64), F32, esq)
                    pti = [0]
                    p.memset(u[:], 0.0, w=[('u', c4) for c4 in range(4)])
                    pend = []
                    LOOK = 3
                    uwsb = [sb(f"uwsb{i}", (128, 260), F32, esq) for i in range(2)]
                    oaccs = [oacc, sb("oacc2", (128, 8, 64), F32, esq)]

                    def push(fn):
                        pend.append(fn)
                        while len(pend) > LOOK:
                            pend.pop(0)()

                    def delay(fn, n):
                        if n == 0:
                            fn()
                        else:
                            push(lambda: delay(fn, n - 1))

                    def flush():
                        while pend:
                            pend.pop(0)()

                    def bc3(ap2, n):
                        return ap2.unsqueeze(2).broadcast_to([ap2.shape[0], ap2.shape[1], n])

                    def attn_tile(j, jl):
                        qs = slice(jl * 128, (jl + 1) * 128)
                        oa = oaccs[j % 2]
                        oak = ('oacc', j % 2)

                        def s_tile(g, lhsT, lkeys, K, extra):
                            hs = slice(4 * g, 4 * g + 4)
                            p.cur = 'S'
                            bank = p.next_bank()
                            rk = lkeys + [('QT', g)] + ([('QTm', g, jl)] if K == 96 else [])
                            p.mm(ps[bank][:, :], lhsT, QT[0:K, hs, qs], True, len(extra) == 0, r=rk, w=[('ps', bank)])
                            for ei, (el, er, ek) in enumerate(extra):
                                p.mm(ps[bank][:, :], el, er, False, ei == len(extra) - 1, r=ek, w=[('ps', bank)])
                            pi = pti[0]
                            pti[0] = (pi + 1) % 4
                            p.act(Pt[pi][:, :], ps[bank][:, :], AF.Exp, r=[('ps', bank)], w=[('Pt', pi)])
                            return pi

                        def gview(g):
                            return gates[:, jl, g * 12:(g + 1) * 12].rearrange("p (h b) -> p h b", b=3)

                        def chain(g):
                            p.cur = 'chain'
                            ucb = 3 + g
                            uc = ps[ucb][:, 0:388].rearrange("p (h c) -> p h c", h=4)
                            gsl = gview(g)
                            p.ts(rz[:, 0, :], uc[:, :, 64], 1.0e-30, None, ALU.max, None, r=[('ps', ucb)], w=['rz0'])
                            p.emit('dve', lambda e: e.reciprocal(rz[:, 0, :], rz[:, 0, :]), r=['rz0'], w=['rz0'])
                            p.stt(score[:, :], uc[:, 0, 65:97], rz[:, 0, 0:1], fb[:, j, :], ALU.mult, ALU.add,
                                  r=[('ps', ucb), 'rz0', 'fb'], w=['score'])
                            for h in range(1, 4):
                                p.stt(score[:, :], uc[:, h, 65:97], rz[:, 0, h:h + 1], score[:, :], ALU.mult, ALU.add,
                                      r=[('ps', ucb), 'rz0', 'score'], w=['score'])
                            p.emit('dve', lambda e: e.max(m8[:, :], score[:, :]), r=['score'], w=['m8'])
                            p.ts(thr[:, :], m8[:, 7:8], -5.0e29, None, ALU.max, None, r=['m8'], w=['thr'])
                            p.ts(TM[:, (j % 2) * 2 + g, 64:96], score[:, :], thr[:, 0:1], NEG, ALU.is_lt, ALU.mult,
                                 r=['score', 'thr'], w=[('TM', (j % 2) * 2 + g)])
                            p.tt(coef[:, 0, :], rz[:, 0, :], gsl[:, :, 0], ALU.mult, r=['rz0', ('gates', jl)], w=['coef0'])
                            p.tt(oa[:, 4 * g:4 * g + 4, :], uc[:, :, 0:64], bc3(coef[:, 0, :], 64), ALU.mult,
                                 r=[('ps', ucb), 'coef0'], w=[oak + (g,)])

                        def combine(g, bi, src, skey, gcol, otmp):
                            p.cur = 'combine'
                            uu = src.rearrange("p (h c) -> p h c", h=4)
                            gsl = gview(g)
                            p.emit('dve', lambda e: e.reciprocal(rz[:, 1 + bi, :], uu[:, :, 64]), r=[skey], w=[('rz', bi)])
                            p.tt(coef[:, 1 + bi, :], rz[:, 1 + bi, :], gsl[:, :, gcol], ALU.mult,
                                 r=[('rz', bi), ('gates', jl)], w=[('coef', bi)])
                            p.tt(otmp[:, :, :], uu[:, :, 0:64], bc3(coef[:, 1 + bi, :], 64), ALU.mult,
                                 r=[skey, ('coef', bi)], w=['otmp'])
                            p.tt(oa[:, 4 * g:4 * g + 4, :], oa[:, 4 * g:4 * g + 4, :], otmp[:, :, :], ALU.add,
                                 r=['otmp', oak + (g,)], w=[oak + (g,)])

                        def finalize():
                            p.cur = 'final'
                            of = oa[:, :, :].rearrange("p h d -> p (h d)")
                            p.act(ojunk[:, :], of, AF.Square, r=[oak + (0,), oak + (1,)], w=['ojunk', 'ssn'],
                                  accum_out=ssn[:, :])
                            p.act(ssn[:, :], ssn[:, :], AF.Ln, r=['ssn'], w=['ssn'], bias=EPS, scale=1.0 / 512)
                            p.act(ssn[:, :], ssn[:, :], AF.Exp, r=['ssn'], w=['ssn'], scale=-0.5)
                            p.ts(onb[:, :], of, ssn[:, 0:1], None, ALU.mult, None, r=[oak + (0,), oak + (1,), 'ssn'],
                                 w=['onb'])
                            delay(finalize_b, 2)

                        def finalize_b():
                            p.cur = 'final'
                            for c in range(4):
                                p.tr(psb[:, 512 + c * 128:512 + (c + 1) * 128], onb[:, c * 128:(c + 1) * 128], identb[:, :],
                                     r=['onb', 'identb'], w=['psb'])
                            for c in range(4):
                                p.ts(mg[:, c, qs], psb[:, 512 + c * 128:512 + (c + 1) * 128],
                                     gnnsaT[:, l * 4 + c:l * 4 + c + 1], None, ALU.mult, None,
                                     r=['psb', 'gnnsaT'], w=[('mg', c)])

                        def part_cmp():
                          for g in range(2):
                            hs = slice(4 * g, 4 * g + 4)
                            pi = s_tile(g, KcmpT[0:64, g, :], ['KcmpT'], 64,
                                        [(sel[0:18, j, :], bc[0:18, hs, :], ['sel', 'bc'])])

                            def pv_c(pi=pi, g=g):
                                p.cur = 'PVc'
                                ucb = 3 + g
                                for h in range(4):
                                    p.mm(ps[ucb][:, h * 97:(h + 1) * 97], Pt[pi][:, h * 128:(h + 1) * 128], VcOv[:, g, :],
                                         h == 0, True, r=[('Pt', pi), 'VcOv'], w=[('ps', ucb)], inc=(h == 3))
                                chain(g)
                            push(pv_c)
                        def part_win():
                          for g in range(2):
                            hs = slice(4 * g, 4 * g + 4)
                            kts = list(range(max(0, j - 2), j + 1))
                            for idx, kt in enumerate(kts):
                                kind = kt - (j - 2)
                                pi = s_tile(g, KwT[0:64, g, kt * 128:(kt + 1) * 128], [('KwT', g, kt // 4)], 64,
                                            [(identb[:, :], wb[:, kind, hs, :], ['identb', 'wb'])])

                                def pv_w(pi=pi, g=g, kt=kt, first=(idx == 0), last=(idx == len(kts) - 1)):
                                    p.cur = 'PVw'
                                    for h in range(4):
                                        p.mm(ps[5][:, h * 65:(h + 1) * 65], Pt[pi][:, h * 128:(h + 1) * 128],
                                             Vaug[:, kt, 1, g, :], first and h == 0, True,
                                             r=[('Pt', pi), ('Vaug', kt)], w=[('ps', 5)], inc=(h == 3))
                                    if last:
                                        p.act(uwsb[g][:, :], ps[5][:, 0:260], AF.Copy, r=[('ps', 5)], w=[('uwsb', g)])
                                push(pv_w)
                        def part_tm():
                          p.cur = 'TM'
                          for g in range(2):
                            hs = slice(4 * g, 4 * g + 4)
                            pc = slice(g * 128, (g + 1) * 128)
                            p.tr(psb[0:96, pc], TM[:, (j % 2) * 2 + g, :], identb[:, :],
                                 r=[('TM', (j % 2) * 2 + g), 'identb'], w=['psb'])
                            p.tt(QT[64:96, hs, qs], psb[64:96, pc].unsqueeze(1).broadcast_to([32, 4, 128]),
                                 bc3(rb31[64:96, hs], 128), ALU.add, r=['psb', 'rb31'], w=[('QTm', g, jl)])

                        def part_sel():
                          for g in range(2):
                            hs = slice(4 * g, 4 * g + 4)
                            for kt in range(0, j + 1):
                                extra = []
                                if kt >= j - 1:
                                    extra = [(identb[:, :], nb[:, kt - (j - 1), hs, :], ['identb', 'nb'])]
                                pi = s_tile(g, KE[0:96, g, kt * 128:(kt + 1) * 128], [('KE', g, kt // 4)], 96, extra)

                                def pv_s(pi=pi, g=g, kt=kt, first=(kt == 0), last=(kt == j)):
                                    p.cur = 'PVs'
                                    for h in range(4):
                                        p.mm(ps[6][:, h * 65:(h + 1) * 65], Pt[pi][:, h * 128:(h + 1) * 128],
                                             Vaug[:, kt, 0, g, :], first and h == 0, True,
                                             r=[('Pt', pi), ('Vaug', kt)], w=[('ps', 6)], inc=(h == 3))
                                    if last:
                                        combine(g, 0, uwsb[g][:, :], ('uwsb', g), 2, otmp)
                                        combine(g, 1, ps[6][:, 0:260], ('ps', 6), 1, otmp)
                                        if g == 1:
                                            finalize()
                                push(pv_s)

                        return part_cmp, part_win, part_tm, part_sel
                    prenorm(l, 0, 0, xn, sq, lnv, rstd)
                    for tg in range(4):
                        b = st['wtm']
                        st['wtm'] = 1 - b
                        p.dma('pool', wtm[b][:, :, 0:24], wview(w_in_d, l, OFF_GL, 24), w=[('wtm', b)])
                        specs = [(wview(w_in_d, l, OFF_Q + 64 * h, 64), 64) for h in range(8)]
                        for c4 in range(4):
                            specs += [(wview(w_in_d, l, OFF_CC + 128 * c4, 128), 128),
                                      (wview(w_in_d, l, OFF_CX + 128 * c4, 128), 128),
                                      (wview(w_in_d, l, OFF_CB + 128 * c4, 128), 128)]
                        ocv = htmp

                        def evac_qc(i, tg_, bank, M):
                            if i < 8:
                                p.act(QT[0:64, i, :], ps[bank][0:64, :], AF.Copy, r=[('ps', bank)], w=[('QT', i // 4)],
                                      scale=0.125)
                                return
                            c4 = (i - 8) // 3
                            k3 = (i - 8) % 3
                            cw = lambda k: convwT[:, (l * 3 + k) * 4 + c4:(l * 3 + k) * 4 + c4 + 1]
                            if k3 == 0:
                                p.act(ccsb[:, :], ps[bank][:, :], AF.Copy, r=[('ps', bank)], w=['ccsb'])
                            elif k3 == 1:
                                p.tt(u[:, c4, 2:514], ccsb[:, :], ps[bank][:, :], ALU.mult, r=['ccsb', ('ps', bank)],
                                     w=[('u', c4)])
                                p.ts(ycv[:, :], u[:, c4, 2:514], cw(0), None, ALU.mult, None, r=[('u', c4), 'convwT'],
                                     w=['ycv'])
                                p.stt(ycv[:, :], u[:, c4, 1:513], cw(1), ycv[:, :], ALU.mult, ALU.add,
                                      r=[('u', c4), 'convwT', 'ycv'], w=['ycv'])
                                p.stt(ycv[:, :], u[:, c4, 0:512], cw(2), ycv[:, :], ALU.mult, ALU.add,
                                      r=[('u', c4), 'convwT', 'ycv'], w=['ycv'])
                                p.cp(u[:, c4, 0:2], u[:, c4, 512:514], r=[('u', c4)], w=[('u', c4)])
                            else:
                                p.tt(ocv[:, c4, :], ycv[:, :], ps[bank][:, :], ALU.mult, r=['ycv', ('ps', bank)],
                                     w=[('htmp', c4)])
                                p.act(sq[:, c4, :], ocv[:, c4, :], AF.Square, r=[('htmp', c4)], w=[('sq', c4)])
                        lin_fm(specs, rhs_xn, 8, 1, evac_qc)
                        p.cur = 'gates'
                        for tt_ in range(4):
                            bank = p.next_bank()
                            for kc in range(8):
                                p.mm(ps[bank][:, 0:24], xn[:, kc, tt_ * 128:(tt_ + 1) * 128], wtm[b][:, kc, 0:24],
                                     kc == 0, kc == 7, r=[('xn', kc), ('wtm', b)], w=[('ps', bank)])
                            p.act(gates[:, tt_, :], ps[bank][:, 0:24], AF.Sigmoid, r=[('ps', bank)], w=[('gates', tt_)])
                        bank = p.next_bank()
                        for c4 in range(4):
                            p.mm(ps[bank][:, :], onesb[:, :], sq[:, c4, :], c4 == 0, c4 == 3, r=['onesb', ('sq', c4)],
                                 w=[('ps', bank)])
                        rstd_from(bank, lnv, rstd, 512)
                        for c4 in range(4):
                            p.stt(mg[:, 4 + c4, :], ocv[:, c4, :], gnconvT[:, l * 4 + c4:l * 4 + c4 + 1], rstd[:, :],
                                  ALU.mult, ALU.mult, r=[('htmp', c4), 'gnconvT', 'rstd'], w=[('mg', 4 + c4)])
                        if tg + 1 < 4:
                            prenorm(l, 0, tg + 1, xn, sq, lnv, rstd)
                        pre_o = prefetch_fm(out_specs(l, w_out_d), 8)
                        parts = [attn_tile(tg * 4 + jl, jl) for jl in range(4)]
                        parts[0][0]()
                        parts[0][1]()
                        parts[0][2]()
                        for k in range(4):
                            if k < 3:
                                parts[k + 1][0]()
                            parts[k][3]()
                            if k < 3:
                                parts[k + 1][1]()
                                parts[k + 1][2]()
                        flush()
                        out_proj(l, 1, tg, w_out_d, 8, lambda kc, tg_: (mg[:, kc, :], ('mg', kc)), htmp, sq, lnv, rstd, pre=pre_o)
                    p.barrier()

        def mem_attn(l, s):
            with ExitStack() as esm:
                alloc_w(esm, 8, nbuf=5)
                memhatT = sb("memhatT", (128, 8, 256), BF16, esm)
                memn = sb("memn", (128, 8, 256), BF16, esm)
                KmT = sb("KmT", (64, 4, 256), BF16, esm)
                Vm = sb("Vm", (128, 2, 4, 65), BF16, esm)
                xn = sb("xn_m", (128, 8, 512), BF16, esm)
                sq = sb("sq_m", (128, 8, 512), BF16, esm)
                lnv = sb("lnv_m", (128, 512), F32, esm)
                rstd = sb("rstd_m", (128, 512), F32, esm)
                QmT = sb("QmT", (64, 4, 512), BF16, esm)
                omT = sb("omT", (128, 2, 512), BF16, esm)
                htmp = sb("htmp_m", (128, 8, 512), F32, esm)
                Pt = [sb(f"Ptm{i}", (128, 512), BF16, esm) for i in range(3)]
                rzm = sb("rzm", (128, 4), F32, esm)
                omb = sb("omb", (128, 256), BF16, esm)
                mst = sb("mst", (128, 1024), F32, esm)
                mss = sb("mss", (128, 1), F32, esm)
                mjunk = sb("mjunk", (128, 1024), BF16, esm)
                mhb = sb("mhb", (128, 1024), BF16, esm)
                pti = [0]
                mpend = []
                for mt in range(2):
                    p.dma('sp', mst[:, :], mem_d[s, mt * 128:(mt + 1) * 128, :], w=['mst'])
                    p.act(mjunk[:, :], mst[:, :], AF.Square, r=['mst'], w=['mjunk', 'mss'], accum_out=mss[:, :])
                    p.act(mss[:, :], mss[:, :], AF.Ln, r=['mss'], w=['mss'], bias=EPS, scale=1.0 / D)
                    p.act(mss[:, :], mss[:, :], AF.Exp, r=['mss'], w=['mss'], scale=-0.5)
                    p.ts(mhb[:, :], mst[:, :], mss[:, 0:1], None, ALU.mult, None, r=['mst', 'mss'], w=['mhb'])
                    for c in range(8):
                        p.tr(psb[:, c * 128:(c + 1) * 128], mhb[:, c * 128:(c + 1) * 128], identb[:, :],
                             r=['mhb', 'identb'], w=['psb'])
                    p.cp(memhatT[:, :, mt * 128:(mt + 1) * 128], psb[:, :].rearrange("p (c t) -> p c t", c=8),
                         r=['psb'], w=['memhatT'])
                for c in range(8):
                    p.ts(memn[:, c, :], memhatT[:, c, :], memnormT[:, l * 8 + c:l * 8 + c + 1], None, ALU.mult, None,
                         r=['memhatT', 'memnormT'], w=[('memn', c)])
                p.memset(Vm[:], 1.0, w=['Vm'])
                specs = [(wview(mwkv_d, l, 64 * h, 64), 64) for h in range(4)]

                def evac_km(i, tg_, bank, M):
                    p.cp(KmT[:, i, :], ps[bank][0:64, 0:256], r=[('ps', bank)], w=['KmT'])
                lin_fm(specs, lambda kc, tg_: (memn[:, kc, :], ('memn', kc)), 8, 1, evac_km, tgw=256)
                b = st['wtm']
                st['wtm'] = 1 - b
                p.dma('pool', wtm[b][:, :, 0:256], wview(mwkv_d, l, 256, 256), w=[('wtm', b)])
                for mt in range(2):
                    bank = p.next_bank()
                    for kc in range(8):
                        p.mm(ps[bank][:, 0:256], memn[:, kc, mt * 128:(mt + 1) * 128], wtm[b][:, kc, :], kc == 0, kc == 7,
                             r=[('memn', kc), ('wtm', b)], w=[('ps', bank)])
                    p.cp(Vm[:, mt, :, 0:64], ps[bank][:, 0:256].rearrange("p (h d) -> p h d", h=4), r=[('ps', bank)],
                         w=['Vm'])
                prenorm(l, 2, 0, xn, sq, lnv, rstd)
                for tg in range(4):
                    if tg + 1 < 4:
                        prenorm(l, 2, tg + 1, xn, sq, lnv, rstd, part='a')
                    specs = [(wview(mwq_d, l, 64 * h, 64), 64) for h in range(4)]

                    def evac_qm(i, tg_, bank, M):
                        p.act(QmT[:, i, :], ps[bank][0:64, :], AF.Copy, r=[('ps', bank)], w=['QmT'], scale=0.125)
                    lin_fm(specs, lambda kc, tg_: (xn[:, kc, :], ('xn', kc)), 8, 1, evac_qm)
                    if tg + 1 < 4:
                        prenorm(l, 2, tg + 1, xn, sq, lnv, rstd, part='b')
                    pre_o = prefetch_fm(out_specs(l, mwo_d), 2)
                    for jl in range(4):
                        qs = slice(jl * 128, (jl + 1) * 128)
                        for mt in range(2):
                            bank = p.next_bank()
                            for h in range(4):
                                p.mm(ps[bank][:, h * 128:(h + 1) * 128], KmT[:, h, mt * 128:(mt + 1) * 128], QmT[:, h, qs],
                                     h == 0, True, r=['KmT', 'QmT'], w=[('ps', bank)], inc=(h == 3))
                            pi = pti[0]
                            pti[0] = (pi + 1) % 3
                            p.act(Pt[pi][:, :], ps[bank][:, :], AF.Exp, r=[('ps', bank)], w=[('Ptm', pi)])

                            def pv_m(pi=pi, mt=mt, qs=qs):
                                for h in range(4):
                                    p.mm(ps[4][:, h * 65:(h + 1) * 65], Pt[pi][:, h * 128:(h + 1) * 128], Vm[:, mt, h, :],
                                         mt == 0 and h == 0, True, r=[('Ptm', pi), 'Vm'], w=[('ps', 4)], inc=(h == 3))
                                if mt == 1:
                                    uu = ps[4][:, 0:260].rearrange("p (h c) -> p h c", h=4)
                                    p.emit('dve', lambda e: e.reciprocal(rzm[:, :], uu[:, :, 64]), r=[('ps', 4)], w=['rzm'])
                                    p.tt(omb[:, :].rearrange("p (h d) -> p h d", h=4), uu[:, :, 0:64],
                                         rzm[:, :].unsqueeze(2).broadcast_to([128, 4, 64]), ALU.mult,
                                         r=[('ps', 4), 'rzm'], w=['omb'])
                                    for c in range(2):
                                        p.tr(psb[:, c * 128:(c + 1) * 128], omb[:, c * 128:(c + 1) * 128], identb[:, :],
                                             r=['omb', 'identb'], w=['psb'])
                                    p.cp(omT[:, :, qs], psb[:, 0:256].rearrange("p (c t) -> p c t", c=2), r=['psb'],
                                         w=[('omT', 0), ('omT', 1)])
                            mpend.append(pv_m)
                            while len(mpend) > 2:
                                mpend.pop(0)()
                    while mpend:
                        mpend.pop(0)()
                    out_proj(l, 3, tg, mwo_d, 2, lambda kc, tg_: (omT[:, kc, :], ('omT', kc)), htmp, sq, lnv, rstd, pre=pre_o)
                p.barrier()

        def ffn(l):
            with ExitStack() as esf:
                alloc_w(esf, KF, with_tm=False, nbuf=4)
                xn = sb("xn_f", (128, 8, 1024), BF16, esf)
                sq = sb("sq_f", (128, 8, 512), BF16, esf)
                lnv = sb("lnv_f", (128, 512), F32, esf)
                rstd = sb("rstd_f", (128, 512), F32, esf)
                hT = sb("hT", (128, KF, 1024), BF16, esf)
                htmp = sb("htmp_f", (128, 8, 512), F32, esf)
                sg = [[sb(f"sg{a}{b_}", (128, 512), F32, esf) for b_ in range(2)] for a in range(2)]
                for sub in range(2):
                    prenorm(l, 4, sub, xn, sq, lnv, rstd, xn_off=sub * 512, tag=str(sub))
                for half in range(2):
                    specs = []
                    for c in range(KF):
                        specs.append((wview(wg_d, l, c * 128, 128), 128))
                        specs.append((wview(wu_d, l, c * 128, 128), 128))

                    def evac_gu(i, sub, bank, M):
                        c = i // 2
                        buf = sg[sub][c % 2]
                        key = ('sg', sub, c % 2)
                        if i % 2 == 0:
                            p.act(buf[:, :], ps[bank][:, :], AF.Silu, r=[('ps', bank)], w=[key])
                        else:
                            p.tt(hT[:, c, sub * 512:(sub + 1) * 512], buf[:, :], ps[bank][:, :], ALU.mult,
                                 r=[key, ('ps', bank)], w=[('hT', c, sub)])
                    lin_fm(specs, lambda kc, sub: (xn[:, kc, sub * 512:(sub + 1) * 512], ('xn' + str(sub), kc)), 8, 2,
                           evac_gu)
                    if half == 0:
                        for sub in range(2):
                            prenorm(l, 4, 2 + sub, xn, sq, lnv, rstd, xn_off=sub * 512, tag=str(sub))
                    for sub in range(2):
                        out_proj(l, 5, half * 2 + sub, wd_d, KF,
                                 lambda kc, tg_, sub=sub: (hT[:, kc, sub * 512:(sub + 1) * 512], ('hT', kc, sub)),
                                 htmp, sq, lnv, rstd)
                p.barrier()

        for s in range(n_seq):
            load_sequence(s)
            for l in layers:
                mixer(l)
                if stop_after == 'mixer':
                    break
                mem_attn(l, s)
                if stop_after == 'mem':
                    break
                ffn(l)
                p.new_sems()
            store_sequence(s)
            p.new_sems()
        print("program instructions:", p.n_ins)
    return nc


_PARAM_KEYS = ("w_in", "w_out", "cmp_k_w1", "cmp_k_w2", "cmp_v_w1", "cmp_v_w2", "cmp_pe", "mem_wq", "mem_wkv", "mem_wo",
               "ffn_wg", "ffn_wu", "ffn_wd")


def make_in_maps(inputs, n_cores, n_seq):
    consts = _host_consts(inputs["rel_bias"])
    shared = dict(consts)
    shared["normsT"] = _fm(inputs["norms"], 8)
    shared["memnormT"] = _fm(inputs["mem_norm"], 8)
    shared["gnnsaT"] = _fm(inputs["gn_nsa"], 4)
    shared["gnconvT"] = _fm(inputs["gn_conv"], 4)
    shared["convwT"] = _fm(inputs["conv_w"], 4)
    for k in _PARAM_KEYS:
        shared[k] = np.ascontiguousarray(np.asarray(inputs[k], np.float32))
    x = np.asarray(inputs["x"], np.float32)
    mem = np.asarray(inputs["mem"], np.float32)
    maps = []
    for c in range(n_cores):
        m = dict(shared)
        m["x"] = np.ascontiguousarray(x[c * n_seq:(c + 1) * n_seq])
        m["mem"] = np.ascontiguousarray(mem[c * n_seq:(c + 1) * n_seq])
        maps.append(m)
    return maps


def kernel(**inputs):
    n_cores = 8
    n_seq = 2
    nc = build_program(n_seq=n_seq, layers=(0, 1))
    maps = make_in_maps(inputs, n_cores, n_seq)
    res = run_bass_kernel_spmd(nc, maps, core_ids=list(range(n_cores)))
    out = np.concatenate([np.asarray(r["y"], np.float32) for r in res.results], axis=0)
    return out
```
